# Optimizing a Trainium2 kernel written in Bass

```python
import jax, jax.numpy as jnp
from jax import lax
import numpy as np

D_MODEL = 1024
BATCH = 32
SEQ = 2048
DEPTH = 4

CHUNK = 64
A_HEADS = 8
A_HEAD_DIM = 64
A_WIDTH = A_HEADS * A_HEAD_DIM
A_LEFT_CHUNKS = 8
A_BAND = (A_LEFT_CHUNKS + 1) * CHUNK
A_MAX_REL = 128
A_N_REL = 2 * A_MAX_REL + 1
CONV_CH = 512
CONV_WIDTH = 31
C_HEADS = 8
C_NOPE = 64
C_ROPE = 32
C_V = 64
C_QK = C_NOPE + C_ROPE
C_WIDTH = C_HEADS * C_V
C_Q_RANK = 384
C_KV_RANK = 256
ROPE_THETA = 10000.0
Q_BLOCK = 128
IN_OFFSETS = (A_WIDTH, 2 * A_WIDTH, 3 * A_WIDTH, 3 * A_WIDTH + 2 * CONV_CH, 3 * A_WIDTH + 2 * CONV_CH + C_Q_RANK, 3 * A_WIDTH + 2 * CONV_CH + C_Q_RANK + C_KV_RANK)
IN_COLS = 3 * A_WIDTH + 2 * CONV_CH + C_Q_RANK + C_KV_RANK + C_ROPE
N_BRANCH = 3
N_EXPERTS = 32
TOP_K = 4
D_EXPERT = 1024
SWIGLU_ALPHA = 1.702
SWIGLU_LIMIT = 7.0
MOE_BLOCK = 256
DN_ALPHA = (2 * DEPTH) ** 0.25
DN_BETA = (8 * DEPTH) ** -0.25
LN_EPS = 1e-5
RMS_EPS = 1e-6
NEG_INF = -1e30

kernel_name = 'hybrid_chunked_attn_conv_mla_moe_deepnorm'


def layer_norm(x, g, b):
    xf = x.astype(jnp.float32)
    mu = jnp.mean(xf, axis=-1, keepdims=True)
    var = jnp.mean(jnp.square(xf - mu), axis=-1, keepdims=True)
    return ((xf - mu) * lax.rsqrt(var + LN_EPS) * g + b).astype(x.dtype)


def rms_norm(x, g):
    xf = x.astype(jnp.float32)
    return (xf * lax.rsqrt(jnp.mean(jnp.square(xf), axis=-1, keepdims=True) + RMS_EPS) * g).astype(x.dtype)


def rope_cos_sin(positions):
    inv = ROPE_THETA ** (-jnp.arange(0, C_ROPE, 2, dtype=jnp.float32) / C_ROPE)
    ang = positions.astype(jnp.float32)[..., None] * inv
    return jnp.cos(ang), jnp.sin(ang)


def apply_rope(x, cos, sin):
    x1, x2 = jnp.split(x, 2, axis=-1)
    return jnp.concatenate([x1 * cos - x2 * sin, x2 * cos + x1 * sin], axis=-1).astype(x.dtype)


def chunked_relpos_attention(q, k, v, rel_table):
    B, S, H, hd = q.shape
    nc = S // CHUNK
    pad = A_LEFT_CHUNKS * CHUNK
    kp = jnp.pad(k, ((0, 0), (pad, 0), (0, 0), (0, 0)))
    vp = jnp.pad(v, ((0, 0), (pad, 0), (0, 0), (0, 0)))
    qc = q.reshape(B, nc, CHUNK, H, hd).transpose(1, 0, 2, 3, 4)
    band_off = jnp.arange(A_BAND) - pad
    rel = band_off[None, :] - jnp.arange(CHUNK)[:, None]
    rel_idx = jnp.clip(rel, -A_MAX_REL, A_MAX_REL) + A_MAX_REL
    bias = rel_table[:, rel_idx].astype(jnp.float32)
    scale = hd ** -0.5

    def one_chunk(args):
        c, q_blk = args
        start = c * CHUNK
        k_band = lax.dynamic_slice_in_dim(kp, start, A_BAND, axis=1)
        v_band = lax.dynamic_slice_in_dim(vp, start, A_BAND, axis=1)
        s = jnp.einsum('bqhd,bkhd->bhqk', q_blk, k_band, preferred_element_type=jnp.float32) * scale + bias
        valid = (start + band_off) >= 0
        s = jnp.where(valid, s, NEG_INF)
        p = jax.nn.softmax(s, axis=-1).astype(v.dtype)
        return jnp.einsum('bhqk,bkhd->bqhd', p, v_band)

    out = lax.map(one_chunk, (jnp.arange(nc), qc))
    return out.transpose(1, 0, 2, 3, 4).reshape(B, S, H * hd)


def conformer_conv(u, w_dw, b_dw, g_ln, b_ln, w_pw2):
    a, gate = jnp.split(u, 2, axis=-1)
    h = a * jax.nn.sigmoid(gate)
    h = lax.conv_general_dilated(h, w_dw[:, None, :], window_strides=(1,), padding=[(CONV_WIDTH - 1, 0)],
                                 dimension_numbers=('NWC', 'WIO', 'NWC'), feature_group_count=CONV_CH) + b_dw
    h = jax.nn.silu(layer_norm(h, g_ln, b_ln))
    return h @ w_pw2


def latent_attention(cq_raw, ckv_raw, kr_raw, g_q, g_kv, w_uq, w_ukv, cos, sin):
    B, S, _ = cq_raw.shape
    q = (rms_norm(cq_raw, g_q) @ w_uq).reshape(B, S, C_HEADS, C_QK)
    q_nope, q_rope = q[..., :C_NOPE], q[..., C_NOPE:]
    q_rope = apply_rope(q_rope, cos[:, :, None, :], sin[:, :, None, :])
    kv = (rms_norm(ckv_raw, g_kv) @ w_ukv).reshape(B, S, C_HEADS, C_NOPE + C_V)
    k_nope, v = kv[..., :C_NOPE], kv[..., C_NOPE:]
    k_rope = apply_rope(kr_raw, cos, sin)
    nqb = S // Q_BLOCK
    qn_b = q_nope.reshape(B, nqb, Q_BLOCK, C_HEADS, C_NOPE).transpose(1, 0, 2, 3, 4)
    qr_b = q_rope.reshape(B, nqb, Q_BLOCK, C_HEADS, C_ROPE).transpose(1, 0, 2, 3, 4)
    key_chunk = jnp.arange(S) // CHUNK
    scale = C_QK ** -0.5

    def one_block(args):
        i, qn, qr = args
        s = (jnp.einsum('bqhd,bkhd->bhqk', qn, k_nope, preferred_element_type=jnp.float32)
             + jnp.einsum('bqhd,bkd->bhqk', qr, k_rope, preferred_element_type=jnp.float32)) * scale
        q_chunk = (i * Q_BLOCK + jnp.arange(Q_BLOCK)) // CHUNK
        s = jnp.where(key_chunk[None, :] <= q_chunk[:, None], s, NEG_INF)
        p = jax.nn.softmax(s, axis=-1).astype(v.dtype)
        return jnp.einsum('bhqk,bkhd->bqhd', p, v)

    out = lax.map(one_block, (jnp.arange(nqb), qn_b, qr_b))
    return out.transpose(1, 0, 2, 3, 4).reshape(B, S, C_WIDTH)


def clamped_swiglu(u):
    glu, lin = jnp.split(u, 2, axis=-1)
    glu = jnp.minimum(glu, SWIGLU_LIMIT)
    lin = jnp.clip(lin, -SWIGLU_LIMIT, SWIGLU_LIMIT)
    return glu * jax.nn.sigmoid(SWIGLU_ALPHA * glu) * (lin + 1.0)


def moe_ffn(h, w_router, b_router, w1, b1, w2, b2):
    B, S, D = h.shape
    N = B * S
    M = N * TOP_K
    xf = h.reshape(N, D)
    logits = (xf @ w_router).astype(jnp.float32) + b_router.astype(jnp.float32)
    top_val, top_idx = lax.top_k(logits, TOP_K)
    gates = jax.nn.softmax(top_val, axis=-1)
    e_flat = top_idx.reshape(-1).astype(jnp.int32)
    g_flat = gates.reshape(-1)
    t_flat = jnp.arange(M, dtype=jnp.int32) // TOP_K
    e_sorted, order = lax.sort((e_flat, jnp.arange(M, dtype=jnp.int32)), num_keys=1, is_stable=True)
    t_sorted = t_flat[order]
    g_sorted = g_flat[order]
    counts = jnp.bincount(e_flat, length=N_EXPERTS)
    padded = (counts + MOE_BLOCK - 1) // MOE_BLOCK * MOE_BLOCK
    start = jnp.cumsum(counts) - counts
    pad_end = jnp.cumsum(padded)
    pad_start = pad_end - padded
    dest = pad_start[e_sorted] + (jnp.arange(M, dtype=jnp.int32) - start[e_sorted])
    n_rows = (M + N_EXPERTS * (MOE_BLOCK - 1) + MOE_BLOCK - 1) // MOE_BLOCK * MOE_BLOCK
    n_blocks = n_rows // MOE_BLOCK
    row_tok = jnp.full((n_rows,), N, jnp.int32).at[dest].set(t_sorted)
    row_gate = jnp.zeros((n_rows,), jnp.float32).at[dest].set(g_sorted)
    block_expert = jnp.minimum(jnp.searchsorted(pad_end, jnp.arange(n_blocks) * MOE_BLOCK, side='right'), N_EXPERTS - 1).astype(jnp.int32)
    x_pad = jnp.concatenate([xf, jnp.zeros((1, D), xf.dtype)], axis=0)

    def one_block(args):
        e, toks, gw = args
        u = x_pad[toks] @ w1[e] + b1[e]
        y = clamped_swiglu(u) @ w2[e] + b2[e]
        return y * gw[:, None].astype(y.dtype)

    y = lax.map(one_block, (block_expert, row_tok.reshape(n_blocks, MOE_BLOCK), row_gate.reshape(n_blocks, MOE_BLOCK)))
    out = jnp.zeros((N + 1, D), y.dtype).at[row_tok].add(y.reshape(n_rows, D))
    return out[:N].reshape(B, S, D).astype(h.dtype)


def setup_inputs(seed: int = 0) -> dict:
    key = jax.random.key(seed)
    ks = jax.random.split(key, 32)
    f32 = jnp.float32
    L, D = DEPTH, D_MODEL

    def w(k, shape, fan_in, scale=1.0):
        return jax.random.normal(k, shape, f32) * (scale * fan_in ** -0.5)

    def gain(k, shape):
        return 1.0 + 0.05 * jax.random.normal(k, shape, f32)

    def small(k, shape, s=0.02):
        return s * jax.random.normal(k, shape, f32)

    x = jax.random.normal(ks[0], (BATCH, SEQ, D), f32)
    offset = jax.random.randint(ks[1], (BATCH, 1), 0, 64, dtype=jnp.int32) * CHUNK
    positions = (offset + jnp.arange(SEQ, dtype=jnp.int32)[None, :]).astype(jnp.int32)
    return {
        'x': x,
        'positions': positions,
        'ln_in_g': gain(ks[2], (D,)),
        'ln_in_b': small(ks[3], (D,)),
        'w_in': w(ks[4], (L, D, IN_COLS), D),
        'w_gate': w(ks[5], (L, D, N_BRANCH * D), D),
        'b_gate': small(ks[6], (L, N_BRANCH * D), 0.01),
        'rel_bias': small(ks[7], (L, A_HEADS, A_N_REL), 0.2),
        'conv_w': w(ks[8], (L, CONV_WIDTH, CONV_CH), CONV_WIDTH),
        'conv_b': small(ks[9], (L, CONV_CH)),
        'conv_ln_g': gain(ks[10], (L, CONV_CH)),
        'conv_ln_b': small(ks[11], (L, CONV_CH)),
        'w_pw2': w(ks[12], (L, CONV_CH, D), CONV_CH),
        'q_norm_g': gain(ks[13], (L, C_Q_RANK)),
        'kv_norm_g': gain(ks[14], (L, C_KV_RANK)),
        'w_uq': w(ks[15], (L, C_Q_RANK, C_HEADS * C_QK), C_Q_RANK),
        'w_ukv': w(ks[16], (L, C_KV_RANK, C_HEADS * (C_NOPE + C_V)), C_KV_RANK),
        'w_oa': w(ks[17], (L, A_WIDTH, D), A_WIDTH),
        'w_oc': w(ks[18], (L, C_WIDTH, D), C_WIDTH),
        'w_out': w(ks[19], (L, D, D), D, DN_BETA),
        'ln1_g': gain(ks[20], (L, D)),
        'ln1_b': small(ks[21], (L, D)),
        'w_router': w(ks[22], (L, D, N_EXPERTS), D),
        'b_router': small(ks[23], (L, N_EXPERTS), 0.01),
        'w1': w(ks[24], (L, N_EXPERTS, D, 2 * D_EXPERT), D),
        'b1': small(ks[25], (L, N_EXPERTS, 2 * D_EXPERT)),
        'w2': w(ks[26], (L, N_EXPERTS, D_EXPERT, D), D_EXPERT, DN_BETA),
        'b2': small(ks[27], (L, N_EXPERTS, D)),
        'ln2_g': gain(ks[28], (L, D)),
        'ln2_b': small(ks[29], (L, D)),
    }


def reference(x, positions, ln_in_g, ln_in_b, w_in, w_gate, b_gate, rel_bias, conv_w, conv_b, conv_ln_g, conv_ln_b,
              w_pw2, q_norm_g, kv_norm_g, w_uq, w_ukv, w_oa, w_oc, w_out, ln1_g, ln1_b, w_router, b_router,
              w1, b1, w2, b2, ln2_g, ln2_b):
    B, S, D = x.shape
    cos, sin = rope_cos_sin(positions)
    h = layer_norm(x, ln_in_g, ln_in_b)
    for l in range(DEPTH):
        proj = h @ w_in[l]
        qa, ka, va, glu_in, cq, ckv, kr = jnp.split(proj, IN_OFFSETS, axis=-1)
        y_a = chunked_relpos_attention(qa.reshape(B, S, A_HEADS, A_HEAD_DIM), ka.reshape(B, S, A_HEADS, A_HEAD_DIM),
                                       va.reshape(B, S, A_HEADS, A_HEAD_DIM), rel_bias[l]) @ w_oa[l]
        y_b = conformer_conv(glu_in, conv_w[l], conv_b[l], conv_ln_g[l], conv_ln_b[l], w_pw2[l])
        y_c = latent_attention(cq, ckv, kr, q_norm_g[l], kv_norm_g[l], w_uq[l], w_ukv[l], cos, sin) @ w_oc[l]
        g_a, g_b, g_c = jnp.split(jax.nn.sigmoid(h @ w_gate[l] + b_gate[l]), N_BRANCH, axis=-1)
        mix = (g_a * y_a + g_b * y_b + g_c * y_c) @ w_out[l]
        h = layer_norm(DN_ALPHA * h + mix, ln1_g[l], ln1_b[l])
        ffn = moe_ffn(h, w_router[l], b_router[l], w1[l], b1[l], w2[l], b2[l])
        h = layer_norm(DN_ALPHA * h + ffn, ln2_g[l], ln2_b[l])
    return h
```

```python
import contextlib
import math
import numpy as np
import concourse.bass as bass
import concourse.mybir as mybir
from concourse.bass_utils import run_bass_kernel_spmd

F32 = mybir.dt.float32
BF16 = mybir.dt.bfloat16
I32 = mybir.dt.int32
U32 = mybir.dt.uint32
AF = mybir.ActivationFunctionType
ALU = mybir.AluOpType
AX = mybir.AxisListType

D = 1024
S = 2048
NT = S // 128
NG = S // 512
IN_COLS = 3232
E = 32
BLK = 512
DN_ALPHA = 8.0 ** 0.25
LN_EPS = 1e-5
RMS_EPS = 1e-6
NEG = -30000.0
SW_ALPHA = 1.702
SW_LIM = 7.0


def _prod(xs):
    r = 1
    for x in xs:
        r *= int(x)
    return r


class Buf:
    __slots__ = ("name", "lw", "rd", "dsem", "dcnt", "excl")

    def __init__(self, name, excl=False):
        self.name = name
        self.excl = excl
        self.lw = None
        self.rd = {}
        self.dsem = None
        self.dcnt = 0


class Sched:
    def __init__(self, nc, stack):
        self.nc = nc
        self.stack = stack
        self.eng = {}
        for nm, h in (("pe", nc.tensor), ("act", nc.scalar), ("dve", nc.vector),
                      ("pool", nc.gpsimd), ("sp", nc.sync)):
            sem = stack.enter_context(nc.semaphore("e_" + nm))
            self.eng[nm] = dict(h=h, sem=sem, cnt=0, waited={})
        self.dbufs = []
        self.free_dsems = []
        self.ninst = 0

    def _wait(self, e, evs):
        best = {}
        for ev in evs:
            if ev is None:
                continue
            sem, val = ev
            k = id(sem)
            if k not in best or best[k][1] < val:
                best[k] = (sem, val)
        En = self.eng[e]
        for k, (sem, val) in best.items():
            if En["waited"].get(k, 0) < val:
                En["h"].wait_ge(sem, val)
                En["waited"][k] = val

    def op(self, e, fn, reads=(), writes=()):
        En = self.eng[e]
        xr = [b for b in reads if b.excl]
        if xr:
            reads = [b for b in reads if not b.excl]
            writes = list(writes) + xr
        evs = []
        for b in reads:
            evs.append(b.lw)
        for b in writes:
            evs.append(b.lw)
            evs.extend(b.rd.values())
        if e == "pe":
            evs = [ev for ev in evs if ev is not None and ev[0] is not En["sem"]]
        self._wait(e, evs)
        ins = fn(En["h"])
        En["cnt"] += 1
        ins.then_inc(En["sem"], 1)
        ev = (En["sem"], En["cnt"])
        for b in writes:
            b.lw = ev
            b.rd = {}
        for b in reads:
            b.rd[id(En["sem"])] = ev
        self.ninst += 1
        return ins

    def _dsem(self, b):
        if b.dsem is None:
            if self.free_dsems:
                b.dsem, b.dcnt = self.free_dsems.pop()
            else:
                b.dsem = self.stack.enter_context(self.nc.semaphore("d%d" % len(self.dbufs) + b.name))
                b.dcnt = 0
            self.dbufs.append(b)
        return b.dsem

    def dma(self, q, out, in_, dst, src=None, fn=None, **kw):
        En = self.eng[q]
        sem = self._dsem(dst)
        evs = []
        if src is not None:
            evs.append(src.lw)
        if dst.lw is not None and dst.lw[0] is not sem:
            evs.append(dst.lw)
        evs.extend(dst.rd.values())
        self._wait(q, evs)
        if fn is None:
            ins = En["h"].dma_start(out=out, in_=in_, **kw)
        else:
            ins = fn(En["h"])
        dst.dcnt += 16
        ins.then_inc(sem, 16)
        ev = (sem, dst.dcnt)
        dst.lw = ev
        dst.rd = {}
        if src is not None:
            src.rd[id(sem)] = ev
        self.ninst += 1
        return ins

    def barrier(self):
        evs = [(En["sem"], En["cnt"]) for En in self.eng.values() if En["cnt"] > 0]
        evs += [(b.dsem, b.dcnt) for b in self.dbufs if b.dcnt > 0]
        evs += [ev for ev in self.free_dsems if ev[1] > 0]
        for e in self.eng:
            self._wait(e, evs)
        for b in self.dbufs:
            self.free_dsems.append((b.dsem, b.dcnt))
            b.dsem = None
            b.lw = None
            b.rd = {}
        self.dbufs = []


class Arena:
    def __init__(self, t, n32):
        self.t = t
        self.n = n32
        self.off = 0
        self.cnt = 0

    def alloc(self, shape, dtype, name=None):
        sz = 2 if dtype == BF16 else 4
        nel = _prod(shape[1:])
        n32 = (nel * sz + 3) // 4
        assert self.off + n32 <= self.n, ("arena overflow", name, self.off, n32, self.n)
        v = self.t[0:shape[0], self.off:self.off + n32]
        self.off += n32
        if dtype != F32:
            v = v.bitcast(dtype)
        if nel != v.shape[1]:
            v = v[:, 0:nel]
        if len(shape) == 3:
            v = v.rearrange("p (a b) -> p a b", a=shape[1])
        elif len(shape) == 4:
            v = v.rearrange("p (a b c) -> p a b c", a=shape[1], b=shape[2])
        self.cnt += 1
        return v, Buf("%s%d" % (name or "t", self.cnt))

    def mark(self):
        return self.off

    def release(self, m):
        self.off = m


class Prog:
    def __init__(self, nseq, depth, dbg=False, stop=None, nexp=E):
        self.stop = stop
        self.E = nexp
        self.nseq = nseq
        self.depth = depth
        self.T = nseq * S
        self.NTT = self.T // 128
        self.NBLK = (4 * self.T + E * (BLK - 1) + BLK - 1) // BLK
        self.NROWS = self.NBLK * BLK
        self.dbg = dbg

    def build(self):
        nc = bass.Bass("TRN2", target_bir_lowering=False)
        self.nc = nc
        T, L = self.T, max(self.depth, 1)
        dt = nc.dram_tensor
        self.x = dt("x", [T, D], F32, kind="ExternalInput").ap()
        self.posrep = dt("posrep", [32, T], I32, kind="ExternalInput").ap()
        self.cvec = dt("cvec", [128, 8], F32, kind="ExternalInput").ap()
        self.ln_in = dt("ln_in", [2, D], F32, kind="ExternalInput").ap()
        self.w_in = dt("w_in", [L, 128, 8, IN_COLS], F32, kind="ExternalInput").ap()
        self.w_gate = dt("w_gate", [L, 128, 8, 3 * D], F32, kind="ExternalInput").ap()
        self.b_gate = dt("b_gate", [L, 128, 24], F32, kind="ExternalInput").ap()
        self.biasq = dt("biasq", [L, 128, 8 * 5 * 128], F32, kind="ExternalInput").ap()
        self.conv_w = dt("conv_w", [L, 128, 4 * 31], F32, kind="ExternalInput").ap()
        self.conv_v = dt("conv_v", [L, 128, 12], F32, kind="ExternalInput").ap()
        self.w_pw2 = dt("w_pw2", [L, 128, 4, D], F32, kind="ExternalInput").ap()
        self.qkv_g = dt("qkv_g", [L, 128, 5], F32, kind="ExternalInput").ap()
        self.w_uq = dt("w_uq", [L, 128, 3, 768], F32, kind="ExternalInput").ap()
        self.w_ukv = dt("w_ukv", [L, 128, 2, 1024], F32, kind="ExternalInput").ap()
        self.w_oa = dt("w_oa", [L, 128, 4, D], F32, kind="ExternalInput").ap()
        self.w_oc = dt("w_oc", [L, 128, 4, D], F32, kind="ExternalInput").ap()
        self.w_out = dt("w_out", [L, 128, 8, D], F32, kind="ExternalInput").ap()
        self.lnp = dt("lnp", [L, 4, D], F32, kind="ExternalInput").ap()
        self.w_router = dt("w_router", [L, 128, 8 * self.E], F32, kind="ExternalInput").ap()
        self.b_router = dt("b_router", [L, self.E], F32, kind="ExternalInput").ap()
        self.w1 = dt("w1", [L * self.E * 1024, 2048], F32, kind="ExternalInput").ap()
        self.b1 = dt("b1", [L * self.E * 128, 16], F32, kind="ExternalInput").ap()
        self.w2 = dt("w2", [L * self.E * 1024, 1024], F32, kind="ExternalInput").ap()
        self.b2 = dt("b2", [L * self.E, 1024], F32, kind="ExternalInput").ap()
        self.out = dt("out", [T, D], F32, kind="ExternalOutput").ap()
        self.h_d = dt("h_d", [T, D], F32, kind="Internal").ap()
        self.h1_d = dt("h1_d", [T, D], F32, kind="Internal").ap()
        self.h1b_d = dt("h1b_d", [T, D], BF16, kind="Internal").ap()
        self.xs_d = dt("xs_d", [self.NROWS, D], BF16, kind="Internal").ap()
        self.ys_d = dt("ys_d", [self.NROWS, D], F32, kind="Internal").ap()
        self.B_h = Buf("h_d")
        self.B_h1 = Buf("h1_d")
        self.B_h1b = Buf("h1b_d")
        self.B_xs = Buf("xs_d")
        self.B_ys = Buf("ys_d")
        self.B_out = Buf("out")
        self.rope_d = dt("rope_d", [2, 32, T], F32, kind="Internal").ap()
        self.B_rope = Buf("rope_d")
        self.aT_d = [dt("aT_d%d" % i, [128, 4, S], BF16, kind="Internal").ap() for i in range(3)]
        self.B_aT = [Buf("aT_d%d" % i) for i in range(3)]
        if self.dbg:
            self.dbg_h = dt("dbg_h", [T, D], F32, kind="ExternalOutput").ap()
            self.dbg_h1 = dt("dbg_h1", [T, D], F32, kind="ExternalOutput").ap()
            self.B_dbg = Buf("dbg")

        with contextlib.ExitStack() as stack:
            self.sc = Sched(nc, stack)
            at = stack.enter_context(nc.sbuf_tensor("arena", [128, 49000], F32))
            self.ar = Arena(at, 49000)
            ct = stack.enter_context(nc.sbuf_tensor("consts", [128, 3400], F32))
            self.car = Arena(ct, 3400)
            self.ps = stack.enter_context(nc.psum_tensor("ps", [128, 8 * 512], F32))
            self.psb = [Buf("ps%d" % i, excl=True) for i in range(8)]
            self.psi = 0
            self.consts()
            self.rope_tables()
            self.ln0()
            for l in range(self.depth):
                self.layer(l)
            if self.depth == 0:
                self.sc.dma("sp", self.out, self.h_d, self.B_out, self.B_h)
            elif self.stop == "mix":
                self.sc.dma("sp", self.out, self.h1_d, self.B_out, self.B_h1)
            elif self.stop is not None:
                self.sc.dma("sp", self.out, self.h_d, self.B_out, self.B_h)
            self.sc.barrier()
        return nc

    def dump(self, name, ap, B):
        o = self.nc.dram_tensor("dbg_" + name, list(ap.shape), ap.dtype, kind="ExternalOutput").ap()
        self.sc.dma("sp", o, ap, Buf("dbg_" + name), B)

    def psum(self, dtype=F32, banks=1, hi=8, fixed=None):
        if fixed is not None:
            i = fixed
        else:
            if self.psi + banks > hi:
                self.psi = 0
            i = self.psi
            self.psi = (self.psi + banks) % hi
        v = self.ps[:, i * 512:(i + banks) * 512]
        if dtype != F32:
            v = v.bitcast(dtype)
        return v, self.psb[i:i + banks]

    def consts(self):
        sc, nc = self.sc, self.nc
        car = self.car
        self.ident_f, self.B_c = car.alloc([128, 128], F32, "identf")
        B = self.B_c
        self.ident_b, _ = car.alloc([128, 128], BF16, "identb")
        self.ones_f, _ = car.alloc([128, 128], F32, "onesf")
        self.ones_b, _ = car.alloc([128, 128], BF16, "onesb")
        self.upper_b, _ = car.alloc([128, 128], BF16, "upper")
        self.cv, _ = car.alloc([128, 8], F32, "cvec")
        self.iota_e, _ = car.alloc([128, E], F32, "iotae")
        self.iota_p, _ = car.alloc([128, 1], F32, "iotap")
        self.lng, self.B_ln = car.alloc([128, D], F32, "lng")
        self.lnb, _ = car.alloc([128, D], F32, "lnb")
        tmpi, _ = car.alloc([128, 128], I32, "tmpi")
        sc.op("pool", lambda h: h.memset(self.ones_f, 1.0), writes=[B])
        sc.op("pool", lambda h: h.memset(self.ones_b, 1.0), writes=[B])
        sc.op("pool", lambda h: h.affine_select(out=self.ident_f, in_=self.ones_f, pattern=[[-1, 128]],
                                                 compare_op=ALU.is_equal, fill=0.0, base=0, channel_multiplier=1),
              reads=[B], writes=[B])
        sc.op("pool", lambda h: h.tensor_copy(out=self.ident_b, in_=self.ident_f), reads=[B], writes=[B])
        sc.op("pool", lambda h: h.affine_select(out=self.upper_b, in_=self.ones_b, pattern=[[1, 128]],
                                                 compare_op=ALU.is_gt, fill=0.0, base=0, channel_multiplier=-1),
              reads=[B], writes=[B])
        sc.op("pool", lambda h: h.iota(tmpi[:, 0:E], pattern=[[1, E]], base=0, channel_multiplier=0), writes=[B])
        sc.op("pool", lambda h: h.tensor_copy(out=self.iota_e, in_=tmpi[:, 0:E]), reads=[B], writes=[B])
        sc.op("pool", lambda h: h.iota(tmpi[:, 0:1], pattern=[[1, 1]], base=0, channel_multiplier=1), writes=[B])
        sc.op("pool", lambda h: h.tensor_copy(out=self.iota_p, in_=tmpi[:, 0:1]), reads=[B], writes=[B])
        sc.dma("sp", self.cv, self.cvec, B)

    def load_ln(self, g_ap, b_ap):
        sc = self.sc
        sc.dma("sp", self.lng, g_ap.partition_broadcast(128), self.B_ln)
        sc.dma("sp", self.lnb, b_ap.partition_broadcast(128), self.B_ln)

    def rsqrt(self, o, Bo, i, Bi, eps, scale):
        sc = self.sc
        sc.op("act", lambda h: h.activation(out=o, in_=i, func=AF.Sqrt, bias=self.eps_ap(eps, o.shape[0]), scale=scale),
              reads=[Bi, self.B_c], writes=[Bo])
        sc.op("dve", lambda h: h.reciprocal(out=o, in_=o), reads=[Bo], writes=[Bo])

    def eps_ap(self, eps, npart):
        i = {LN_EPS: 6, RMS_EPS: 7}[eps]
        return self.cv[0:npart, i:i + 1]

    def ln_tile(self, z, Bz, o, Bo, st, Bst):
        sc = self.sc
        stats = st[:, 0:12].rearrange("p (a b) -> p a b", a=2)
        for c in range(2):
            sc.op("dve", lambda h, c=c: h.bn_stats(out=stats[:, c, :], in_=z[:, c * 512:(c + 1) * 512]),
                  reads=[Bz], writes=[Bst])
        mv = st[:, 12:14]
        sc.op("dve", lambda h: h.bn_aggr(out=mv, in_=stats), reads=[Bst], writes=[Bst])
        rstd = st[:, 14:15]
        self.rsqrt(rstd, Bst, mv[:, 1:2], Bst, LN_EPS, 1.0)
        sc.op("dve", lambda h: h.tensor_scalar(out=o, in0=z, scalar1=mv[:, 0:1], scalar2=rstd,
                                               op0=ALU.subtract, op1=ALU.mult), reads=[Bz, Bst], writes=[Bo])
        sc.op("pool", lambda h: h.tensor_tensor(out=o, in0=o, in1=self.lng, op=ALU.mult),
              reads=[Bo, self.B_ln], writes=[Bo])
        sc.op("pool", lambda h: h.tensor_tensor(out=o, in0=o, in1=self.lnb, op=ALU.add),
              reads=[Bo, self.B_ln], writes=[Bo])

    def ln0(self):
        sc, ar = self.sc, self.ar
        m = ar.mark()
        self.load_ln(self.ln_in[0], self.ln_in[1])
        xt = [ar.alloc([128, D], F32, "x") for _ in range(2)]
        ot = [ar.alloc([128, D], F32, "o") for _ in range(2)]
        st = [ar.alloc([128, 16], F32, "st") for _ in range(2)]
        for t in range(self.NTT):
            (z, Bz), (o, Bo), (s_, Bs) = xt[t % 2], ot[t % 2], st[t % 2]
            sc.dma("sp", z, self.x[t * 128:(t + 1) * 128, :], Bz)
            self.ln_tile(z, Bz, o, Bo, s_, Bs)
            sc.dma("sp", self.h_d[t * 128:(t + 1) * 128, :], o, self.B_h, Bo)
        sc.barrier()
        ar.release(m)


    def mm(self, ps, Bps, lhsT, rhs, start, stop, reads):
        self.sc.op("pe", lambda h: h.matmul(ps, lhsT=lhsT, rhs=rhs, start=start, stop=stop),
                   reads=reads, writes=Bps)

    def wload(self, q, dst, Bd, src):
        n = dst.shape[-1]
        if dst.dtype == F32:
            self.sc.dma("sp", dst, src, Bd)
            return
        for c0 in range(0, n, 2048):
            c1 = min(n, c0 + 2048)
            if len(dst.shape) == 3:
                self.sc.dma("pool", dst[:, :, c0:c1], src[:, :, c0:c1], Bd)
            else:
                self.sc.dma("pool", dst[:, c0:c1], src[:, c0:c1], Bd)

    def rope_tables(self):
        sc, ar, nc = self.sc, self.ar, self.nc
        m = ar.mark()
        T = self.T
        CH = 2048
        for c0 in range(0, T, CH):
            pi_, Bp = ar.alloc([96, CH], I32, "posi")
            ang, Ba = ar.alloc([96, CH], F32, "ang")
            r, Br = ar.alloc([96, CH], F32, "r")
            o, Bo = ar.alloc([96, CH], F32, "o")
            sc.dma("sp", pi_[64:96, :], self.posrep[:, c0:c0 + CH], Bp)
            sc.op("dve", lambda h: h.tensor_copy(out=ang[64:96, :], in_=pi_[64:96, :]), reads=[Bp], writes=[Ba])
            sc.op("dve", lambda h: h.tensor_scalar(out=ang[64:96, :], in0=ang[64:96, :], scalar1=self.cv[64:96, 0:1],
                                                   scalar2=None, op0=ALU.mult), reads=[Ba, self.B_c], writes=[Ba])
            ki, Bk = ar.alloc([96, CH], I32, "ki")
            kf, Bkf = ar.alloc([96, CH], F32, "kf")
            cc, Bcc = ar.alloc([96, CH], F32, "cc")
            P_ = slice(64, 96)
            TWO_PI = 2 * math.pi
            PI_LO = 3.1415925
            for j, sh in enumerate((0.5 * math.pi, 0.0)):
                sc.op("dve", lambda h, sh=sh: h.tensor_scalar(out=r[P_, :], in0=ang[P_, :], scalar1=sh, scalar2=None, op0=ALU.add),
                      reads=[Ba], writes=[Br])
                sc.op("dve", lambda h: h.tensor_scalar(out=ki[P_, :], in0=r[P_, :], scalar1=1.0 / TWO_PI, scalar2=None, op0=ALU.mult),
                      reads=[Br], writes=[Bk])
                sc.op("dve", lambda h: h.tensor_copy(out=kf[P_, :], in_=ki[P_, :]), reads=[Bk], writes=[Bkf])
                sc.op("dve", lambda h: h.scalar_tensor_tensor(out=r[P_, :], in0=kf[P_, :], scalar=-TWO_PI, in1=r[P_, :],
                                                              op0=ALU.mult, op1=ALU.add), reads=[Bkf, Br], writes=[Br])
                sc.op("dve", lambda h: h.tensor_scalar(out=cc[P_, :], in0=r[P_, :], scalar1=math.pi, scalar2=-TWO_PI,
                                                       op0=ALU.is_gt, op1=ALU.mult), reads=[Br], writes=[Bcc])
                sc.op("dve", lambda h: h.tensor_tensor(out=r[P_, :], in0=r[P_, :], in1=cc[P_, :], op=ALU.add), reads=[Br, Bcc], writes=[Br])
                sc.op("dve", lambda h: h.tensor_scalar(out=cc[P_, :], in0=r[P_, :], scalar1=-math.pi, scalar2=TWO_PI,
                                                       op0=ALU.is_lt, op1=ALU.mult), reads=[Br], writes=[Bcc])
                sc.op("dve", lambda h: h.tensor_tensor(out=r[P_, :], in0=r[P_, :], in1=cc[P_, :], op=ALU.add), reads=[Br, Bcc], writes=[Br])
                sc.op("dve", lambda h: h.tensor_scalar(out=r[P_, :], in0=r[P_, :], scalar1=PI_LO, scalar2=-PI_LO,
                                                       op0=ALU.min, op1=ALU.max), reads=[Br], writes=[Br])
                sc.op("act", lambda h: h.activation(out=o[P_, :], in_=r[P_, :], func=AF.Sin), reads=[Br], writes=[Bo])
                sc.dma("sp", self.rope_d[j, :, c0:c0 + CH], o[64:96, :], self.B_rope, Bo)
            ar.release(m)
        sc.barrier()

    def layer(self, l):
        if self.stop == "rope":
            return
        for s in range(self.nseq):
            self.mixer_seq(l, s)
        self.sc.barrier()
        if self.stop is not None and self.stop != "route":
            return
        self.moe(l)

    def mixer_seq(self, l, s):
        sc, ar = self.sc, self.ar
        t0 = s * S
        m0 = ar.mark()
        hT, B_hT = ar.alloc([128, 8, S], BF16, "hT")
        mA = ar.mark()
        cqn, B_cq = ar.alloc([128, 3, S], BF16, "cqn")
        ckvn, B_ckv = ar.alloc([128, 2, S], BF16, "ckvn")
        krR, B_kr = ar.alloc([96, S], BF16, "krR")
        m_b3 = ar.mark()
        glu, B_glu = ar.alloc([128, 4, 30 + S], BF16, "glu")
        m_b2 = ar.mark()
        qkT, B_qk = ar.alloc([128, 8, S], BF16, "qkT")
        vA, B_vA = ar.alloc([128, NT, 8, 65], BF16, "vA")
        m_b1 = ar.mark()
        wbuf, B_w = ar.alloc([128, 8, 1696], BF16, "win")
        wkr, B_wkr = ar.alloc([128, 8, 96], BF16, "wkr")
        gq, B_gq = ar.alloc([128, 5], F32, "gq")
        mT = ar.mark()
        sc.dma("sp", gq, self.qkv_g[l], B_gq)
        sc.op("pool", lambda h: h.memset(glu[:, :, 0:30], 0.0), writes=[B_glu])
        sc.op("pool", lambda h: h.memset(vA[:, :, :, 64:65], 1.0), writes=[B_vA])
        hx = [ar.alloc([128, D], F32, "hx") for _ in range(2)]
        hb = [ar.alloc([128, D], BF16, "hb") for _ in range(2)]
        for tt in range(NT):
            (x_, Bx), (b_, Bb) = hx[tt % 2], hb[tt % 2]
            sc.dma("sp", x_, self.h_d[t0 + tt * 128:t0 + (tt + 1) * 128, :], Bx, self.B_h)
            sc.op("act", lambda h: h.copy(out=b_, in_=x_), reads=[Bx], writes=[Bb])
            ps, Bps = self.psum(BF16)
            for k in range(8):
                sc.op("pe", lambda h, k=k: h.transpose(ps[:, k * 128:(k + 1) * 128], b_[:, k * 128:(k + 1) * 128], self.ident_b),
                      reads=[Bb, self.B_c], writes=Bps)
            sc.op("dve", lambda h: h.tensor_copy(out=hT[:, :, tt * 128:(tt + 1) * 128],
                                                 in_=ps.rearrange("p (k n) -> p k n", k=8)),
                  reads=Bps, writes=[B_hT])
        ar.release(mT)
        if self.stop == "A0":
            sc.barrier()
            return
        tmp = [ar.alloc([128, 512], F32, "tmp") for _ in range(8)]
        tb = [ar.alloc([128, 512], BF16, "tb") for _ in range(2)]
        rc, B_rc = ar.alloc([96, 512], F32, "ropec")
        rs, B_rs = ar.alloc([96, 512], F32, "ropes")
        ti = [0]

        def T_():
            ti[0] += 1
            return tmp[ti[0] % 8]

        def fm_proj(col0, M, g, wb=wbuf, Bw=B_w):
            ps, Bps = self.psum()
            for k in range(8):
                self.mm(ps[0:M, :], Bps, wb[:, k, col0:col0 + M], hT[:, k, g * 512:(g + 1) * 512],
                        k == 0, k == 7, [Bw, B_hT])
            return ps, Bps

        self.wload("pool", wbuf[:, :, 0:1536], B_w, self.w_in[l, :, :, 0:1536])
        for g in range(NG):
            gs = slice(g * 512, (g + 1) * 512)
            for c in range(8):
                ps, Bps = fm_proj(c * 128, 128, g)
                if c < 4:
                    sc.op("act", lambda h, ps=ps, c=c: h.activation(out=qkT[:, c, gs], in_=ps, func=AF.Copy, scale=0.125),
                          reads=Bps, writes=[B_qk])
                else:
                    sc.op("dve", lambda h, ps=ps, c=c: h.tensor_copy(out=qkT[:, c, gs], in_=ps), reads=Bps, writes=[B_qk])
        for tt in range(NT):
            ps, Bps = self.psum()
            for k in range(8):
                self.mm(ps, Bps, hT[:, k, tt * 128:(tt + 1) * 128], wbuf[:, k, 1024:1536], k == 0, k == 7, [B_w, B_hT])
            sc.op("act" if tt % 2 else "dve",
                  (lambda h, ps=ps, tt=tt: h.copy(out=vA[:, tt, :, 0:64], in_=ps.rearrange("p (a b) -> p a b", a=8))) if tt % 2 else
                  (lambda h, ps=ps, tt=tt: h.tensor_copy(out=vA[:, tt, :, 0:64], in_=ps.rearrange("p (a b) -> p a b", a=8))),
                  reads=Bps, writes=[B_vA])
        if self.stop == "A1":
            sc.barrier()
            return
        self.wload("pool", wbuf[:, :, 0:1696], B_w, self.w_in[l, :, :, 1536:3232])
        sc.op("pool", lambda h: h.tensor_copy(out=wkr[:, :, 0:64], in_=wbuf[:, :, 1600:1664]), reads=[B_w], writes=[B_wkr])
        sc.op("pool", lambda h: h.tensor_scalar(out=wkr[:, :, 64:80], in0=wbuf[:, :, 1680:1696], scalar1=-1.0, scalar2=None,
                                                op0=ALU.mult), reads=[B_w], writes=[B_wkr])
        sc.op("pool", lambda h: h.tensor_copy(out=wkr[:, :, 80:96], in_=wbuf[:, :, 1664:1680]), reads=[B_w], writes=[B_wkr])
        for g in range(NG):
            gs = slice(g * 512, (g + 1) * 512)
            for c in range(4):
                pa, Bpa = fm_proj(c * 128, 128, g)
                pg, Bpg = fm_proj(512 + c * 128, 128, g)
                sg, Bsg = T_()
                sc.op("act", lambda h, pg=pg, sg=sg: h.activation(out=sg, in_=pg, func=AF.Sigmoid), reads=Bpg, writes=[Bsg])
                sc.op("dve", lambda h, pa=pa, sg=sg, c=c: h.tensor_tensor(out=glu[:, c, 30 + g * 512:30 + (g + 1) * 512],
                                                                         in0=pa, in1=sg, op=ALU.mult),
                      reads=Bpa + [Bsg], writes=[B_glu])
            if self.stop == "A2":
                sc.barrier()
                return
            for (c0, nt_, gcol, dstT, Bdst, nfeat) in ((1024, 3, 0, cqn, B_cq, 384), (1408, 2, 3, ckvn, B_ckv, 256)):
                raws = []
                pss, Bpss = None, None
                for j in range(nt_):
                    ps, Bps = fm_proj(c0 + j * 128, 128, g)
                    raw, Braw = T_()
                    sq, Bsq = T_()
                    sq = sq.bitcast(BF16)[:, 0:512]
                    sc.op("dve", lambda h, ps=ps, raw=raw: h.tensor_copy(out=raw, in_=ps), reads=Bps, writes=[Braw])
                    sc.op("act", lambda h, ps=ps, sq=sq: h.activation(out=sq, in_=ps, func=AF.Square), reads=Bps, writes=[Bsq])
                    raws.append((raw, Braw, sq, Bsq))
                pss, Bpss = self.psum()
                for j in range(nt_):
                    self.mm(pss, Bpss, self.ones_b, raws[j][2], j == 0, j == nt_ - 1, [self.B_c, raws[j][3]])
                rb, Brb = T_()
                sc.op("act", lambda h, pss=pss, rb=rb, nfeat=nfeat: h.activation(out=rb, in_=pss, func=AF.Sqrt,
                                                                             bias=self.eps_ap(RMS_EPS, 128), scale=1.0 / nfeat),
                      reads=Bpss + [self.B_c], writes=[Brb])
                sc.op("dve", lambda h, rb=rb: h.reciprocal(out=rb, in_=rb), reads=[Brb], writes=[Brb])
                for j in range(nt_):
                    raw, Braw = raws[j][0], raws[j][1]
                    sc.op("dve", lambda h, raw=raw, j=j, rb=rb, dstT=dstT, gcol=gcol:
                          h.scalar_tensor_tensor(out=dstT[:, j, gs], in0=raw, scalar=gq[:, gcol + j:gcol + j + 1], in1=rb,
                                                 op0=ALU.mult, op1=ALU.mult),
                          reads=[Braw, Brb, B_gq], writes=[Bdst])
            if self.stop == "A3":
                sc.barrier()
                return
            sc.dma("sp", rc[64:96, :], self.rope_d[0, :, t0 + g * 512:t0 + (g + 1) * 512], B_rc, self.B_rope)
            sc.dma("sp", rs[64:96, :], self.rope_d[1, :, t0 + g * 512:t0 + (g + 1) * 512], B_rs, self.B_rope)
            pa, Bpa = fm_proj(1600, 96, g)
            pb, Bpb = fm_proj(0, 96, g, wb=wkr, Bw=B_wkr)
            t1, Bt1 = T_()
            t2, Bt2 = T_()
            sc.op("dve", lambda h, pa=pa, t1=t1: h.tensor_tensor(out=t1[64:96, :], in0=pa[64:96, :], in1=rc[64:96, :], op=ALU.mult),
                  reads=Bpa + [B_rc], writes=[Bt1])
            sc.op("dve", lambda h, pb=pb, t2=t2: h.tensor_tensor(out=t2[64:96, :], in0=pb[64:96, :], in1=rs[64:96, :], op=ALU.mult),
                  reads=Bpb + [B_rs], writes=[Bt2])
            sc.op("pool", lambda h, t1=t1, t2=t2: h.tensor_tensor(out=krR[64:96, gs], in0=t1[64:96, :], in1=t2[64:96, :], op=ALU.add),
                  reads=[Bt1, Bt2], writes=[B_kr])
        sc.barrier()
        ar.release(m_b1)
        if self.stop == "A":
            self.dump("qkT", qkT, B_qk)
            self.dump("vA", vA, B_vA)
            self.dump("glu", glu, B_glu)
            self.dump("cqn", cqn, B_cq)
            self.dump("ckvn", ckvn, B_ckv)
            self.dump("krR", krR[64:96, :], B_kr)
            self.dump("hT", hT, B_hT)
            sc.barrier()
            return
        self.attn_a(l, s, qkT, B_qk, vA, B_vA)
        sc.barrier()
        ar.release(m_b2)
        if self.stop == "B1":
            return
        self.conv_b(l, s, glu, B_glu)
        sc.barrier()
        ar.release(m_b3)
        if self.stop == "B2":
            return
        self.attn_c(l, s, cqn, B_cq, ckvn, B_ckv, krR, B_kr)
        sc.barrier()
        ar.release(mA)
        if self.stop == "B3":
            for i in range(3):
                self.dump("aT%d" % i, self.aT_d[i], self.B_aT[i])
            sc.barrier()
            return
        self.mix_c(l, s, hT, B_hT)
        sc.barrier()
        ar.release(m0)

    def tr4(self, src, Bsrc, dst, Bdst, qi):
        sc = self.sc
        pt, Bpt = self.psum(BF16, hi=6)
        for j in range(4):
            sc.op("pe", lambda h, j=j: h.transpose(pt[:, j * 128:(j + 1) * 128], src[:, j * 128:(j + 1) * 128], self.ident_b),
                  reads=[Bsrc, self.B_c], writes=Bpt)
        sc.op("dve", lambda h: h.tensor_copy(out=dst[:, :, qi * 128:(qi + 1) * 128],
                                             in_=pt[:, 0:512].rearrange("p (k n) -> p k n", k=4)),
              reads=Bpt, writes=[Bdst])

    def attn_a(self, l, s, qkT, B_qk, vA, B_vA):
        sc, ar = self.sc, self.ar
        m = ar.mark()
        bq, B_bq = ar.alloc([128, 8, 5, 128], BF16, "bq")
        self.wload("pool", bq.rearrange("p a b c -> p (a b c)"), B_bq, self.biasq[l])
        Et = [ar.alloc([128, 5, 128], BF16, "E") for _ in range(2)]
        at = [ar.alloc([128, 512], BF16, "at") for _ in range(2)]
        rcp = [ar.alloc([128, 4], F32, "rcp") for _ in range(2)]
        aT, B_aT = ar.alloc([128, 4, S], BF16, "aTA")
        ec = 0
        for qi in range(NT):
            a_, Ba = at[qi % 2]
            kts = [kt for kt in range(qi - 4, qi + 1) if kt >= 0]
            nk = len(kts)
            for half in range(2):
                po, Bpo = self.psum(fixed=6 + half)
                pov = po[:, 0:260].rearrange("p (a b) -> p a b", a=4)
                for hh in range(4):
                    h_ = half * 4 + hh
                    tq = h_ // 2
                    pr = (h_ % 2) * 64
                    pS, BpS = self.psum(banks=2, hi=6)
                    for i, kt in enumerate(kts):
                        j = kt - qi + 4
                        o_ = pS[:, i * 128:(i + 1) * 128]
                        self.mm(o_, BpS, qkT[pr:pr + 64, 4 + tq, kt * 128:(kt + 1) * 128],
                                qkT[pr:pr + 64, tq, qi * 128:(qi + 1) * 128], True, False, [B_qk])
                        self.mm(o_, BpS, bq[:, h_, j, :], self.ident_b, False, True, [B_bq, self.B_c])
                    E_, BE = Et[ec % 2]
                    ec += 1
                    sc.op("act", lambda h, E_=E_, pS=pS, nk=nk: h.activation(
                        out=E_[:, 0:nk, :], in_=pS[:, 0:nk * 128].rearrange("p (a b) -> p a b", a=nk), func=AF.Exp),
                        reads=BpS, writes=[BE])
                    for i, kt in enumerate(kts):
                        self.mm(pov[:, hh, :], Bpo, E_[:, i, :], vA[:, kt, h_, :], i == 0, i == nk - 1, [BE, B_vA])
                r_, Br = rcp[half]
                sc.op("dve", lambda h, r_=r_, pov=pov: h.reciprocal(out=r_, in_=pov[:, :, 64]), reads=Bpo, writes=[Br])
                sc.op("dve", lambda h, r_=r_, pov=pov, half=half, a_=a_: h.tensor_tensor(
                    out=a_[:, half * 256:(half + 1) * 256].rearrange("p (a b) -> p a b", a=4), in0=pov[:, :, 0:64],
                    in1=r_.unsqueeze(2).to_broadcast([128, 4, 64]), op=ALU.mult), reads=Bpo + [Br], writes=[Ba])
            self.tr4(a_, Ba, aT, B_aT, qi)
        sc.dma("sp", self.aT_d[0], aT, self.B_aT[0], B_aT)
        sc.barrier()
        ar.release(m)

    def conv_b(self, l, s, glu, B_glu):
        sc, ar = self.sc, self.ar
        m = ar.mark()
        cw, B_cw = ar.alloc([128, 4, 31], F32, "cw")
        cvv, B_cv = ar.alloc([128, 12], F32, "cvv")
        sc.dma("sp", cw.rearrange("p a b -> p (a b)"), self.conv_w[l], B_cw)
        sc.dma("sp", cvv, self.conv_v[l], B_cv)
        xc = [ar.alloc([128, S], F32, "xc") for _ in range(4)]
        cT, B_cT = ar.alloc([128, 4, S], BF16, "cT")
        tmp = [ar.alloc([128, 512], F32, "ctmp") for _ in range(8)]
        ti = [0]

        def T_():
            ti[0] += 1
            return tmp[ti[0] % 8]
        for ct in range(4):
            e = "dve"
            x_, Bx = xc[ct]
            sc.op(e, lambda h, x_=x_, ct=ct: h.tensor_scalar(out=x_, in0=glu[:, ct, 0:S], scalar1=cw[:, ct, 0:1],
                                                            scalar2=cvv[:, ct:ct + 1], op0=ALU.mult, op1=ALU.add),
                  reads=[B_glu, B_cw, B_cv], writes=[Bx])
            for k in range(1, 31):
                sc.op(e, lambda h, x_=x_, ct=ct, k=k: h.scalar_tensor_tensor(out=x_, in0=glu[:, ct, k:k + S], scalar=cw[:, ct, k:k + 1],
                                                                            in1=x_, op0=ALU.mult, op1=ALU.add),
                      reads=[B_glu, B_cw, Bx], writes=[Bx])
        for g in range(NG):
            gs = slice(g * 512, (g + 1) * 512)
            pm, Bpm = self.psum()
            pq, Bpq = self.psum()
            for ct in range(4):
                x_, Bx = xc[ct]
                sq, Bsq = T_()
                sc.op("act", lambda h, x_=x_, sq=sq: h.activation(out=sq, in_=x_[:, gs], func=AF.Square), reads=[Bx], writes=[Bsq])
                self.mm(pm, Bpm, self.ones_f, x_[:, gs], ct == 0, ct == 3, [self.B_c, Bx])
                self.mm(pq, Bpq, self.ones_f, sq, ct == 0, ct == 3, [self.B_c, Bsq])
            mean, Bm = T_()
            msq, Bq = T_()
            rstd, Br = T_()
            sc.op("act", lambda h: h.activation(out=mean, in_=pm, func=AF.Copy, scale=1.0 / 512), reads=Bpm, writes=[Bm])
            sc.op("dve", lambda h: h.tensor_tensor(out=msq, in0=mean, in1=mean, op=ALU.mult), reads=[Bm], writes=[Bq])
            sc.op("dve", lambda h: h.scalar_tensor_tensor(out=rstd, in0=pq, scalar=1.0 / 512, in1=msq, op0=ALU.mult, op1=ALU.subtract),
                  reads=Bpq + [Bq], writes=[Br])
            self.rsqrt(rstd, Br, rstd, Br, LN_EPS, 1.0)
            for ct in range(4):
                x_, Bx = xc[ct]
                d, Bd = T_()
                sc.op("dve", lambda h, x_=x_, d=d: h.tensor_tensor(out=d, in0=x_[:, gs], in1=mean, op=ALU.subtract),
                      reads=[Bx, Bm], writes=[Bd])
                sc.op("pool", lambda h, d=d: h.tensor_tensor(out=d, in0=d, in1=rstd, op=ALU.mult), reads=[Bd, Br], writes=[Bd])
                sc.op("act", lambda h, d=d, ct=ct: h.activation(out=cT[:, ct, gs], in_=d, func=AF.Silu,
                                                              bias=cvv[:, 8 + ct:9 + ct], scale=cvv[:, 4 + ct:5 + ct]),
                      reads=[Bd, B_cv], writes=[B_cT])
        sc.dma("sp", self.aT_d[1], cT, self.B_aT[1], B_cT)
        sc.barrier()
        ar.release(m)

    def attn_c(self, l, s, cqn, B_cq, ckvn, B_ckv, krR, B_kr):
        sc, ar = self.sc, self.ar
        t0 = s * S
        m = ar.mark()
        wq, B_wq = ar.alloc([128, 3, 768], BF16, "wq")
        wr, B_wr = ar.alloc([128, 3, 8, 96], BF16, "wr")
        wkv, B_wkv = ar.alloc([128, 2, 1024], BF16, "wkv")
        self.wload("pool", wq, B_wq, self.w_uq[l])
        self.wload("pool", wkv, B_wkv, self.w_ukv[l])
        wq4 = wq.rearrange("p k (a b) -> p k a b", a=8)
        for k in range(3):
            sc.op("pool", lambda h, k=k: h.tensor_copy(out=wr[:, k, :, 0:64], in_=wq4[:, k, :, 0:64]), reads=[B_wq], writes=[B_wr])
            sc.op("pool", lambda h, k=k: h.tensor_scalar(out=wr[:, k, :, 64:80], in0=wq4[:, k, :, 80:96], scalar1=-1.0, scalar2=None,
                                                        op0=ALU.mult), reads=[B_wq], writes=[B_wr])
            sc.op("pool", lambda h, k=k: h.tensor_copy(out=wr[:, k, :, 80:96], in_=wq4[:, k, :, 64:80]), reads=[B_wq], writes=[B_wr])
        rc, B_rc = ar.alloc([96, S], F32, "rc")
        rs, B_rs = ar.alloc([96, S], F32, "rs")
        sc.dma("sp", rc[64:96, :], self.rope_d[0, :, t0:t0 + S], B_rc, self.B_rope)
        sc.dma("sp", rs[64:96, :], self.rope_d[1, :, t0:t0 + S], B_rs, self.B_rope)
        Qh = [ar.alloc([96, S], BF16, "Qh") for _ in range(2)]
        Kh = [ar.alloc([96, S], BF16, "Kh") for _ in range(2)]
        Vh = [ar.alloc([128, NT, 65], BF16, "Vh") for _ in range(2)]
        Es = [ar.alloc([128, 16, 512], BF16, "Ec") for _ in range(2)]
        at, B_at = ar.alloc([128, NT, 512], BF16, "atC")
        aT, B_aT = ar.alloc([128, 4, S], BF16, "aTC")
        tmp = [ar.alloc([96, 512], F32, "mtmp") for _ in range(4)]
        rcp = [ar.alloc([128, 1], F32, "rcpc") for _ in range(4)]
        for i in range(2):
            sc.op("pool", lambda h, i=i: h.memset(Vh[i][0][:, :, 64:65], 1.0), writes=[Vh[i][1]])
        scale = 96.0 ** -0.5
        ec = 0
        tc_ = 0
        rcnt = 0
        for h_ in range(8):
            Q_, BQ = Qh[h_ % 2]
            K_, BK = Kh[h_ % 2]
            V_, BV = Vh[h_ % 2]
            for g in range(NG):
                gs = slice(g * 512, (g + 1) * 512)
                pa, Bpa = self.psum()
                pb, Bpb = self.psum()
                for k in range(3):
                    self.mm(pa[0:96, :], Bpa, wq[:, k, h_ * 96:(h_ + 1) * 96], cqn[:, k, gs], k == 0, k == 2, [B_wq, B_cq])
                for k in range(3):
                    self.mm(pb[0:96, :], Bpb, wr[:, k, h_, :], cqn[:, k, gs], k == 0, k == 2, [B_wr, B_cq])
                sc.op("act", lambda h, pa=pa, Q_=Q_: h.copy(out=Q_[0:64, gs], in_=pa[0:64, :]), reads=Bpa, writes=[BQ])
                t1, Bt1 = tmp[tc_ % 4]
                t2, Bt2 = tmp[(tc_ + 1) % 4]
                tc_ += 2
                sc.op("dve", lambda h, pa=pa, t1=t1: h.tensor_tensor(out=t1[64:96, :], in0=pa[64:96, :], in1=rc[64:96, gs], op=ALU.mult),
                      reads=Bpa + [B_rc], writes=[Bt1])
                sc.op("dve", lambda h, pb=pb, t2=t2: h.tensor_tensor(out=t2[64:96, :], in0=pb[64:96, :], in1=rs[64:96, gs], op=ALU.mult),
                      reads=Bpb + [B_rs], writes=[Bt2])
                sc.op("pool", lambda h, t1=t1, t2=t2, Q_=Q_: h.tensor_tensor(out=Q_[64:96, gs], in0=t1[64:96, :], in1=t2[64:96, :], op=ALU.add),
                      reads=[Bt1, Bt2], writes=[BQ])
                pk, Bpk = self.psum()
                for k in range(2):
                    self.mm(pk[0:64, :], Bpk, wkv[:, k, h_ * 128:h_ * 128 + 64], ckvn[:, k, gs], k == 0, k == 1, [B_wkv, B_ckv])
                sc.op("dve", lambda h, pk=pk, K_=K_: h.tensor_copy(out=K_[0:64, gs], in_=pk[0:64, :]), reads=Bpk, writes=[BK])
            sc.op("pool", lambda h, K_=K_: h.tensor_copy(out=K_[64:96, :], in_=krR[64:96, :]), reads=[B_kr], writes=[BK])
            for t8 in range(NT // 8):
                pv, Bpv = self.psum()
                for ti_ in range(8):
                    tt = t8 * 8 + ti_
                    for k in range(2):
                        self.mm(pv[:, ti_ * 64:(ti_ + 1) * 64], Bpv, ckvn[:, k, tt * 128:(tt + 1) * 128],
                                wkv[:, k, h_ * 128 + 64:h_ * 128 + 128], k == 0, k == 1, [B_wkv, B_ckv])
                sc.op("act", lambda h, pv=pv, V_=V_, t8=t8: h.copy(out=V_[:, t8 * 8:(t8 + 1) * 8, 0:64],
                                                                  in_=pv.rearrange("p (a b) -> p a b", a=8)),
                      reads=Bpv, writes=[BV])
            for G in range(NG):
                E_, BE = Es[ec % 2]
                ec += 1
                nkt = 4 * G + 4
                for kt in range(nkt):
                    pS, BpS = self.psum()
                    self.mm(pS, BpS, K_[0:96, kt * 128:(kt + 1) * 128], Q_[0:96, G * 512:(G + 1) * 512], True, True, [BK, BQ])
                    sc.op("act", lambda h, pS=pS, E_=E_, kt=kt: h.activation(out=E_[:, kt, :], in_=pS, func=AF.Exp, scale=scale),
                          reads=BpS, writes=[BE])
                    if kt >= 4 * G:
                        ql = kt - 4 * G
                        sc.op("pool", lambda h, E_=E_, kt=kt, ql=ql: h.memset(E_[64:128, kt, ql * 128:ql * 128 + 64], 0.0), writes=[BE])
                for ql in range(4):
                    qi = 4 * G + ql
                    po, Bpo = self.psum()
                    for kt in range(qi + 1):
                        self.mm(po[:, 0:65], Bpo, E_[:, kt, ql * 128:(ql + 1) * 128], V_[:, kt, :], kt == 0, kt == qi, [BE, BV])
                    r_, Br = rcp[rcnt % 4]
                    rcnt += 1
                    sc.op("dve", lambda h, r_=r_, po=po: h.reciprocal(out=r_, in_=po[:, 64:65]), reads=Bpo, writes=[Br])
                    sc.op("dve", lambda h, r_=r_, po=po, qi=qi, h_=h_: h.tensor_scalar(
                        out=at[:, qi, h_ * 64:(h_ + 1) * 64], in0=po[:, 0:64], scalar1=r_, scalar2=None, op0=ALU.mult),
                        reads=Bpo + [Br], writes=[B_at])
        for qi in range(NT):
            self.tr4(at[:, qi, :], B_at, aT, B_aT, qi)
        sc.dma("sp", self.aT_d[2], aT, self.B_aT[2], B_aT)
        sc.barrier()
        ar.release(m)

    def mix_c(self, l, s, hT, B_hT):
        sc, ar = self.sc, self.ar
        t0 = s * S
        m = ar.mark()
        wgs = [ar.alloc([128, 8, D], BF16, "wg") for _ in range(3)]
        wo = [ar.alloc([128, 4, D], BF16, "wo") for _ in range(3)]
        wout, B_wout = ar.alloc([128, 8, D], BF16, "wout")
        bg, B_bg = ar.alloc([128, 24], F32, "bg")
        for br, src in enumerate((self.w_oa, self.w_pw2, self.w_oc)):
            self.wload("pool", wo[br][0], wo[br][1], src[l])
            self.wload("pool", wgs[br][0], wgs[br][1], self.w_gate[l, :, :, br * D:(br + 1) * D])
        self.wload("pool", wout, B_wout, self.w_out[l])
        sc.dma("sp", bg, self.b_gate[l], B_bg)
        self.load_ln(self.lnp[l, 0], self.lnp[l, 1])
        aTg = [ar.alloc([128, 4, 512], BF16, "aTg") for _ in range(3)]
        mT, B_mT = ar.alloc([128, 8, 512], BF16, "mT")
        macc = [ar.alloc([128, 512], F32, "macc") for _ in range(8)]
        sgs = [ar.alloc([128, 512], F32, "sg") for _ in range(2)]
        tms = [ar.alloc([128, 512], F32, "tm") for _ in range(2)]
        ht = [ar.alloc([128, D], F32, "ht") for _ in range(2)]
        zt = [ar.alloc([128, D], F32, "zt") for _ in range(1)]
        ot = [ar.alloc([128, D], F32, "ot") for _ in range(2)]
        obt = [ar.alloc([128, D], BF16, "obt") for _ in range(1)]
        stt = [ar.alloc([128, 16], F32, "stt") for _ in range(2)]
        cnt = 0
        for g in range(NG):
            gs = slice(g * 512, (g + 1) * 512)
            for br in range(3):
                sc.dma("sp", aTg[br][0], self.aT_d[br][:, :, gs], aTg[br][1], self.B_aT[br])
            for br in range(3):
                wg, B_wg = wgs[br]
                for c in range(8):
                    ma, Bma = macc[c]
                    py, Bpy = self.psum()
                    for k in range(4):
                        self.mm(py, Bpy, wo[br][0][:, k, c * 128:(c + 1) * 128], aTg[br][0][:, k, :], k == 0, k == 3,
                                [wo[br][1], aTg[br][1]])
                    pg, Bpg = self.psum()
                    for k in range(8):
                        self.mm(pg, Bpg, wg[:, k, c * 128:(c + 1) * 128], hT[:, k, gs], k == 0, k == 7, [B_wg, B_hT])
                    sg, Bsg = sgs[cnt % 2]
                    tm, Btm = tms[cnt % 2]
                    cnt += 1
                    sc.op("act", lambda h, sg=sg, pg=pg, br=br, c=c: h.activation(out=sg, in_=pg, func=AF.Sigmoid,
                                                                               bias=bg[:, br * 8 + c:br * 8 + c + 1], scale=1.0),
                          reads=Bpg + [B_bg], writes=[Bsg])
                    if br == 0:
                        sc.op("dve", lambda h, ma=ma, py=py, sg=sg: h.tensor_tensor(out=ma, in0=py, in1=sg, op=ALU.mult),
                              reads=Bpy + [Bsg], writes=[Bma])
                    else:
                        sc.op("dve", lambda h, tm=tm, py=py, sg=sg: h.tensor_tensor(out=tm, in0=py, in1=sg, op=ALU.mult),
                              reads=Bpy + [Bsg], writes=[Btm])
                        if br == 1:
                            sc.op("pool", lambda h, ma=ma, tm=tm: h.tensor_tensor(out=ma, in0=ma, in1=tm, op=ALU.add),
                                  reads=[Bma, Btm], writes=[Bma])
                        else:
                            sc.op("pool", lambda h, ma=ma, tm=tm, c=c: h.tensor_tensor(out=mT[:, c, :], in0=ma, in1=tm, op=ALU.add),
                                  reads=[Bma, Btm], writes=[B_mT])
            for ql in range(4):
                tt = g * 4 + ql
                r0 = t0 + tt * 128
                (h_, Bh), (z, Bz), (o, Bo), (ob, Bob), (st, Bst) = ht[tt % 2], zt[0], ot[tt % 2], obt[0], stt[tt % 2]
                sc.dma("sp", h_, self.h_d[r0:r0 + 128, :], Bh, self.B_h)
                for n in range(2):
                    pz, Bpz = self.psum()
                    for c in range(8):
                        self.mm(pz, Bpz, mT[:, c, ql * 128:(ql + 1) * 128], wout[:, c, n * 512:(n + 1) * 512], c == 0, c == 7,
                                [B_mT, B_wout])
                    sc.op("dve", lambda h, z=z, h_=h_, pz=pz, n=n: h.scalar_tensor_tensor(
                        out=z[:, n * 512:(n + 1) * 512], in0=h_[:, n * 512:(n + 1) * 512], scalar=DN_ALPHA, in1=pz,
                        op0=ALU.mult, op1=ALU.add), reads=[Bh] + Bpz, writes=[Bz])
                self.ln_tile(z, Bz, o, Bo, st, Bst)
                sc.dma("sp", self.h1_d[r0:r0 + 128, :], o, self.B_h1, Bo)
                sc.op("act", lambda h, ob=ob, o=o: h.copy(out=ob, in_=o), reads=[Bo], writes=[Bob])
                sc.dma("sp", self.h1b_d[r0:r0 + 128, :], ob, self.B_h1b, Bob)
        sc.barrier()
        ar.release(m)

    def hs_scan(self, bufs, n, view):
        sc = self.sc
        cur = 0
        sft = 1
        while sft < n:
            (a, Ba), (b, Bb) = bufs[cur], bufs[1 - cur]
            sc.op("dve", lambda h, a=a, b=b, sft=sft: h.tensor_tensor(out=view(b, sft, n), in0=view(a, sft, n), in1=view(a, 0, n - sft),
                                                                     op=ALU.add), reads=[Ba], writes=[Bb])
            sc.op("dve", lambda h, a=a, b=b, sft=sft: h.tensor_copy(out=view(b, 0, sft), in_=view(a, 0, sft)), reads=[Ba], writes=[Bb])
            cur = 1 - cur
            sft *= 2
        return cur

    def moe(self, l):
        sc, ar, nc = self.sc, self.ar, self.nc
        E_, T, NTT, NBLK = self.E, self.T, self.NTT, self.NBLK
        last = (l == self.depth - 1)
        m0 = ar.mark()
        idx4, B_idx4 = ar.alloc([128, NTT, 4], U32, "idx4")
        gate4, B_g4 = ar.alloc([128, NTT, 4], F32, "gate4")
        idxw, B_idxw = ar.alloc([128, NBLK, 8], U32, "idxw")
        idxb1, B_ib1 = ar.alloc([128, NBLK], U32, "idxb1")
        idxb2, B_ib2 = ar.alloc([128, NBLK], U32, "idxb2")
        m1 = ar.mark()
        wr32, B_wr = ar.alloc([128, 8, E_], F32, "wr32")
        brt, B_brt = ar.alloc([128, E_], F32, "brt")
        sc.dma("sp", wr32.rearrange("p a b -> p (a b)"), self.w_router[l], B_wr)
        sc.dma("sp", brt, self.b_router[l].partition_broadcast(128), B_brt)
        mask, B_mask = ar.alloc([128, NTT, E_], F32, "mask")
        topv, B_topv = ar.alloc([128, NTT, 8], F32, "topv")
        eid, B_eid = ar.alloc([128, NTT, 8], U32, "eid")
        hx = [ar.alloc([128, D], F32, "mhx") for _ in range(2)]
        hT32 = [ar.alloc([128, 8, 128], F32, "hT32") for _ in range(2)]
        lgs = [ar.alloc([128, E_], F32, "lg") for _ in range(2)]
        for tt in range(NTT):
            (x_, Bx), (t_, Bt), (lg, Blg) = hx[tt % 2], hT32[tt % 2], lgs[tt % 2]
            sc.dma("sp", x_, self.h1_d[tt * 128:(tt + 1) * 128, :], Bx, self.B_h1)
            for hf in range(2):
                ps, Bps = self.psum()
                for k in range(4):
                    kk = hf * 4 + k
                    sc.op("pe", lambda h, ps=ps, k=k, kk=kk, x_=x_: h.transpose(ps[:, k * 128:(k + 1) * 128], x_[:, kk * 128:(kk + 1) * 128],
                                                                             self.ident_f), reads=[Bx, self.B_c], writes=Bps)
                sc.op("act" if hf else "dve",
                      (lambda h, ps=ps, t_=t_, hf=hf: h.copy(out=t_[:, hf * 4:(hf + 1) * 4, :], in_=ps.rearrange("p (a b) -> p a b", a=4))) if hf else
                      (lambda h, ps=ps, t_=t_, hf=hf: h.tensor_copy(out=t_[:, hf * 4:(hf + 1) * 4, :], in_=ps.rearrange("p (a b) -> p a b", a=4))),
                      reads=Bps, writes=[Bt])
            pl, Bpl = self.psum()
            for k in range(8):
                self.mm(pl[:, 0:E_], Bpl, t_[:, k, :], wr32[:, k, :], k == 0, k == 7, [Bt, B_wr])
            sc.op("dve", lambda h, lg=lg, pl=pl: h.tensor_tensor(out=lg, in0=pl[:, 0:E_], in1=brt, op=ALU.add), reads=Bpl + [B_brt], writes=[Blg])
            sc.op("dve", lambda h, lg=lg, tt=tt: h.max(out=topv[:, tt, :], in_=lg), reads=[Blg], writes=[B_topv])
            sc.op("dve", lambda h, lg=lg, tt=tt: h.max_index(out=eid[:, tt, :], in_max=topv[:, tt, :], in_values=lg),
                  reads=[Blg, B_topv], writes=[B_eid])
            sc.op("dve", lambda h, lg=lg, tt=tt: h.tensor_scalar(out=mask[:, tt, :], in0=lg, scalar1=topv[:, tt, 3:4], scalar2=None,
                                                                op0=ALU.is_ge), reads=[Blg, B_topv], writes=[B_mask])
        d4, B_d4 = ar.alloc([128, NTT, 4], F32, "d4")
        s4, B_s4 = ar.alloc([128, NTT], F32, "s4")
        sc.op("dve", lambda h: h.tensor_tensor(out=d4, in0=topv[:, :, 0:4], in1=topv[:, :, 0:1].to_broadcast([128, NTT, 4]), op=ALU.subtract),
              reads=[B_topv], writes=[B_d4])
        sc.op("act", lambda h: h.activation(out=d4, in_=d4, func=AF.Exp), reads=[B_d4], writes=[B_d4])
        sc.op("dve", lambda h: h.reduce_sum(out=s4, in_=d4, axis=AX.X), reads=[B_d4], writes=[B_s4])
        sc.op("dve", lambda h: h.reciprocal(out=s4, in_=s4), reads=[B_s4], writes=[B_s4])
        sc.op("dve", lambda h: h.tensor_tensor(out=gate4, in0=d4, in1=s4.unsqueeze(2).to_broadcast([128, NTT, 4]), op=ALU.mult),
              reads=[B_d4, B_s4], writes=[B_g4])
        NC_ = NTT * E_
        maskb, B_mb = ar.alloc([128, NC_], BF16, "maskb")
        pre, B_pre = ar.alloc([128, NTT, E_], F32, "pre")
        tot = [ar.alloc([128, NTT, E_], F32, "tot") for _ in range(2)]
        tot0, B_tot0 = ar.alloc([128, NTT, E_], F32, "tot0")
        mflat = mask.rearrange("p a b -> p (a b)")
        sc.op("dve", lambda h: h.tensor_copy(out=maskb, in_=mflat), reads=[B_mask], writes=[B_mb])
        for c0 in range(0, NC_, 512):
            c1 = min(NC_, c0 + 512)
            pp, Bpp = self.psum()
            pt_, Bpt = self.psum()
            self.mm(pp[:, 0:c1 - c0], Bpp, self.upper_b, maskb[:, c0:c1], True, True, [self.B_c, B_mb])
            self.mm(pt_[:, 0:c1 - c0], Bpt, self.ones_b, maskb[:, c0:c1], True, True, [self.B_c, B_mb])
            sc.op("dve", lambda h, pp=pp, c0=c0, c1=c1: h.tensor_copy(out=pre.rearrange("p a b -> p (a b)")[:, c0:c1], in_=pp[:, 0:c1 - c0]),
                  reads=Bpp, writes=[B_pre])
            sc.op("act", lambda h, pt_=pt_, c0=c0, c1=c1: h.copy(out=tot0.rearrange("p a b -> p (a b)")[:, c0:c1], in_=pt_[:, 0:c1 - c0]),
                  reads=Bpt, writes=[B_tot0])
        sc.op("dve", lambda h: h.tensor_copy(out=tot[0][0], in_=tot0), reads=[B_tot0], writes=[tot[0][1]])
        cur = self.hs_scan(tot, NTT, lambda a, lo, hi: a[:, lo:hi, :])
        inc, B_inc = tot[cur]
        cnt, B_cnt = ar.alloc([128, E_], F32, "cnt")
        sc.op("dve", lambda h: h.tensor_copy(out=cnt, in_=inc[:, NTT - 1, :]), reads=[B_inc], writes=[B_cnt])
        posd, B_pos = ar.alloc([128, NTT, E_], F32, "posd")
        sc.op("dve", lambda h: h.tensor_tensor(out=posd, in0=inc, in1=tot0, op=ALU.subtract), reads=[B_inc, B_tot0], writes=[B_pos])
        sc.op("dve", lambda h: h.tensor_tensor(out=posd, in0=posd, in1=pre, op=ALU.add), reads=[B_pos, B_pre], writes=[B_pos])
        J = T // BLK
        ij, B_ij = ar.alloc([128, J], I32, "ij")
        thr, B_thr = ar.alloc([128, J], F32, "thr")
        sc.op("pool", lambda h: h.iota(ij, pattern=[[1, J]], base=0, channel_multiplier=0), writes=[B_ij])
        sc.op("dve", lambda h: h.tensor_copy(out=thr, in_=ij), reads=[B_ij], writes=[B_thr])
        sc.op("dve", lambda h: h.tensor_scalar(out=thr, in0=thr, scalar1=float(BLK), scalar2=None, op0=ALU.mult), reads=[B_thr], writes=[B_thr])
        cmpj, B_cmpj = ar.alloc([128, E_, J], F32, "cmpj")
        sc.op("dve", lambda h: h.tensor_tensor(out=cmpj, in0=cnt.unsqueeze(2).to_broadcast([128, E_, J]),
                                               in1=thr.unsqueeze(1).to_broadcast([128, E_, J]), op=ALU.is_gt),
              reads=[B_cnt, B_thr], writes=[B_cmpj])
        pe_ = [ar.alloc([128, E_], F32, "pe") for _ in range(2)]
        nbk, B_nbk = ar.alloc([128, E_], F32, "nbk")
        sc.op("dve", lambda h: h.reduce_sum(out=nbk, in_=cmpj, axis=AX.X), reads=[B_cmpj], writes=[B_nbk])
        sc.op("dve", lambda h: h.tensor_scalar(out=nbk, in0=nbk, scalar1=float(BLK), scalar2=None, op0=ALU.mult), reads=[B_nbk], writes=[B_nbk])
        sc.op("dve", lambda h: h.tensor_copy(out=pe_[0][0], in_=nbk), reads=[B_nbk], writes=[pe_[0][1]])
        cur = self.hs_scan(pe_, E_, lambda a, lo, hi: a[:, lo:hi])
        pend, B_pend = pe_[cur]
        pst, B_pst = pe_[1 - cur]
        sc.op("dve", lambda h: h.tensor_tensor(out=pst, in0=pend, in1=nbk, op=ALU.subtract), reads=[B_pend, B_nbk], writes=[B_pst])
        sc.op("dve", lambda h: h.tensor_tensor(out=posd, in0=posd, in1=pst.unsqueeze(1).to_broadcast([128, NTT, E_]), op=ALU.add),
              reads=[B_pos, B_pst], writes=[B_pos])
        eidf, B_eidf = ar.alloc([128, NTT, 4], F32, "eidf")
        sc.op("dve", lambda h: h.tensor_copy(out=eidf, in_=eid[:, :, 0:4]), reads=[B_eid], writes=[B_eidf])
        eq, B_eq = ar.alloc([128, NTT, E_], F32, "eq")
        d4f, B_d4f = ar.alloc([128, NTT, 4], F32, "d4f")
        for k in range(4):
            sc.op("dve", lambda h, k=k: h.tensor_tensor(out=eq, in0=self.iota_e[:, 0:E_].unsqueeze(1).to_broadcast([128, NTT, E_]),
                                                        in1=eidf[:, :, k:k + 1].to_broadcast([128, NTT, E_]), op=ALU.is_equal),
                  reads=[self.B_c, B_eidf], writes=[B_eq])
            sc.op("dve", lambda h: h.tensor_tensor(out=eq, in0=eq, in1=posd, op=ALU.mult), reads=[B_eq, B_pos], writes=[B_eq])
            sc.op("dve", lambda h, k=k: h.reduce_sum(out=d4f[:, :, k], in_=eq, axis=AX.X), reads=[B_eq], writes=[B_d4f])
        sc.op("dve", lambda h: h.tensor_copy(out=idx4, in_=d4f), reads=[B_d4f], writes=[B_idx4])
        ib, B_ib = ar.alloc([128, NBLK], I32, "ib")
        bst, B_bst = ar.alloc([128, NBLK], F32, "bst")
        sc.op("pool", lambda h: h.iota(ib, pattern=[[1, NBLK]], base=0, channel_multiplier=0), writes=[B_ib])
        sc.op("dve", lambda h: h.tensor_copy(out=bst, in_=ib), reads=[B_ib], writes=[B_bst])
        sc.op("dve", lambda h: h.tensor_scalar(out=bst, in0=bst, scalar1=float(BLK), scalar2=None, op0=ALU.mult), reads=[B_bst], writes=[B_bst])
        cmpb, B_cmpb = ar.alloc([128, NBLK, E_], F32, "cmpb")
        sc.op("dve", lambda h: h.tensor_tensor(out=cmpb, in0=pend.unsqueeze(1).to_broadcast([128, NBLK, E_]),
                                               in1=bst.unsqueeze(2).to_broadcast([128, NBLK, E_]), op=ALU.is_le),
              reads=[B_pend, B_bst], writes=[B_cmpb])
        bex, B_bex = ar.alloc([128, NBLK], F32, "bex")
        sc.op("dve", lambda h: h.reduce_sum(out=bex, in_=cmpb, axis=AX.X), reads=[B_cmpb], writes=[B_bex])
        sc.op("dve", lambda h: h.tensor_scalar(out=bex, in0=bex, scalar1=float(E_ - 1), scalar2=float(l * E_), op0=ALU.min, op1=ALU.add),
              reads=[B_bex], writes=[B_bex])
        sc.op("dve", lambda h: h.tensor_copy(out=idxb2, in_=bex), reads=[B_bex], writes=[B_ib2])
        t1_, B_t1 = ar.alloc([128, NBLK], F32, "t1_")
        sc.op("dve", lambda h: h.tensor_scalar(out=t1_, in0=bex, scalar1=128.0, scalar2=self.iota_p[:, 0:1], op0=ALU.mult, op1=ALU.add),
              reads=[B_bex, self.B_c], writes=[B_t1])
        sc.op("dve", lambda h: h.tensor_copy(out=idxb1, in_=t1_), reads=[B_t1], writes=[B_ib1])
        sc.op("dve", lambda h: h.tensor_scalar(out=t1_, in0=bex, scalar1=1024.0, scalar2=self.iota_p[:, 0:1], op0=ALU.mult, op1=ALU.add),
              reads=[B_bex, self.B_c], writes=[B_t1])
        t8, B_t8 = ar.alloc([128, NBLK, 8], F32, "t8")
        for k in range(8):
            sc.op("dve", lambda h, k=k: h.tensor_scalar(out=t8[:, :, k], in0=t1_, scalar1=float(k * 128), scalar2=None, op0=ALU.add),
                  reads=[B_t1], writes=[B_t8])
        sc.op("dve", lambda h: h.tensor_copy(out=idxw, in_=t8), reads=[B_t8], writes=[B_idxw])
        if self.stop == "route":
            self.dump("idx4", idx4, B_idx4)
            self.dump("gate4", gate4, B_g4)
            self.dump("idxw", idxw, B_idxw)
            self.dump("cnt", cnt, B_cnt)
            sc.barrier()
            ar.release(m0)
            return
        sc.barrier()
        ar.release(m1)
        hb = [ar.alloc([128, D], BF16, "shb") for _ in range(3)]
        for tt in range(NTT):
            b_, Bb = hb[tt % 3]
            sc.dma("sp", b_, self.h1b_d[tt * 128:(tt + 1) * 128, :], Bb, self.B_h1b)
            for k in range(4):
                sc.dma("pool", None, None, self.B_xs, Bb,
                       fn=lambda h, b_=b_, tt=tt, k=k: h.indirect_dma_start(
                           out=self.xs_d, out_offset=bass.IndirectOffsetOnAxis(ap=idx4[:, tt, k:k + 1], axis=0),
                           in_=b_, in_offset=None))
        sc.barrier()
        ar.release(m1)
        W1 = [ar.alloc([128, 8, 2048], BF16, "W1") for _ in range(2)]
        W2 = [ar.alloc([128, 8, D], BF16, "W2") for _ in range(2)]
        b1t = [ar.alloc([128, 16], F32, "b1t") for _ in range(2)]
        b2t = [ar.alloc([128, D], F32, "b2t") for _ in range(2)]
        b1ps = [ar.alloc([128, 8], F32, "b1p") for _ in range(2)]
        xr = [ar.alloc([128, D], BF16, "xr") for _ in range(4)]
        xT = [ar.alloc([128, 8, BLK], BF16, "xT") for _ in range(2)]
        aT, B_aT = ar.alloc([128, 8, BLK], BF16, "aTm")
        tmp = [ar.alloc([128, 512], F32, "etmp") for _ in range(6)]
        yo = [ar.alloc([128, D], F32, "yo") for _ in range(2)]
        ti = [0]

        def T_():
            ti[0] += 1
            return tmp[ti[0] % 6]
        yc = 0

        def prep_x(b):
            xT_, BxT = xT[b % 2]
            for r in range(4):
                x_, Bx = xr[r]
                r0 = b * BLK + r * 128
                sc.dma("pool", x_, self.xs_d[r0:r0 + 128, :], Bx, self.B_xs)
                ps, Bps = self.psum(BF16)
                for k in range(8):
                    sc.op("pe", lambda h, ps=ps, k=k, x_=x_: h.transpose(ps[:, k * 128:(k + 1) * 128], x_[:, k * 128:(k + 1) * 128], self.ident_b),
                          reads=[Bx, self.B_c], writes=Bps)
                if r % 2:
                    sc.op("dve", lambda h, ps=ps, r=r: h.tensor_copy(out=xT_[:, :, r * 128:(r + 1) * 128], in_=ps.rearrange("p (k n) -> p k n", k=8)),
                          reads=Bps, writes=[BxT])
                else:
                    sc.op("act", lambda h, ps=ps, r=r: h.copy(out=xT_[:, :, r * 128:(r + 1) * 128], in_=ps.rearrange("p (k n) -> p k n", k=8)),
                          reads=Bps, writes=[BxT])
        for b in range(NBLK):
            (w1, Bw1), (w2, Bw2), (b1_, Bb1), (b2_, Bb2), (xT_, BxT) = W1[b % 2], W2[b % 2], b1t[b % 2], b2t[b % 2], xT[b % 2]
            for k in range(8):
                sc.dma("pool", None, None, Bw1, None, fn=lambda h, w1=w1, b=b, k=k: h.indirect_dma_start(
                    out=w1[:, k, :], out_offset=None, in_=self.w1,
                    in_offset=bass.IndirectOffsetOnAxis(ap=idxw[:, b, k:k + 1], axis=0)))
            for k in range(8):
                sc.dma("pool", None, None, Bw2, None, fn=lambda h, w2=w2, b=b, k=k: h.indirect_dma_start(
                    out=w2[:, k, :], out_offset=None, in_=self.w2,
                    in_offset=bass.IndirectOffsetOnAxis(ap=idxw[:, b, k:k + 1], axis=0)))
            sc.dma("pool", None, None, Bb1, None, fn=lambda h, b1_=b1_, b=b: h.indirect_dma_start(
                out=b1_, out_offset=None, in_=self.b1, in_offset=bass.IndirectOffsetOnAxis(ap=idxb1[:, b:b + 1], axis=0)))
            sc.dma("pool", None, None, Bb2, None, fn=lambda h, b2_=b2_, b=b: h.indirect_dma_start(
                out=b2_, out_offset=None, in_=self.b2, in_offset=bass.IndirectOffsetOnAxis(ap=idxb2[:, b:b + 1], axis=0)))
            b1p, Bb1p = b1ps[b % 2]
            sc.op("dve", lambda h, b1p=b1p, b1_=b1_: h.tensor_scalar(out=b1p, in0=b1_[:, 8:16], scalar1=1.0, scalar2=None, op0=ALU.add),
                  reads=[Bb1], writes=[Bb1p])
            if b == 0:
                prep_x(0)
            for j in range(8):
                pg, Bpg = self.psum()
                pl, Bpl = self.psum()
                for k in range(8):
                    self.mm(pg, Bpg, w1[:, k, j * 128:(j + 1) * 128], xT_[:, k, :], k == 0, k == 7, [Bw1, BxT])
                for k in range(8):
                    self.mm(pl, Bpl, w1[:, k, 1024 + j * 128:1024 + (j + 1) * 128], xT_[:, k, :], k == 0, k == 7, [Bw1, BxT])
                gc, Bgc = T_()
                sg, Bsg = T_()
                l1, Bl1 = T_()
                sc.op("dve", lambda h, gc=gc, pg=pg, j=j: h.tensor_scalar(out=gc, in0=pg, scalar1=b1_[:, j:j + 1], scalar2=SW_LIM,
                                                                         op0=ALU.add, op1=ALU.min), reads=Bpg + [Bb1], writes=[Bgc])
                sc.op("act", lambda h, gc=gc, sg=sg: h.activation(out=sg, in_=gc, func=AF.Sigmoid, scale=SW_ALPHA), reads=[Bgc], writes=[Bsg])
                sc.op("dve", lambda h, l1=l1, pl=pl, j=j: h.tensor_scalar(out=l1, in0=pl, scalar1=b1p[:, j:j + 1], scalar2=SW_LIM + 1.0,
                                                                         op0=ALU.add, op1=ALU.min), reads=Bpl + [Bb1p], writes=[Bl1])
                sc.op("dve", lambda h, gc=gc, sg=sg: h.tensor_tensor(out=gc, in0=gc, in1=sg, op=ALU.mult), reads=[Bgc, Bsg], writes=[Bgc])
                sc.op("dve", lambda h, gc=gc, l1=l1, j=j: h.scalar_tensor_tensor(out=aT[:, j, :], in0=l1, scalar=1.0 - SW_LIM, in1=gc,
                                                                                op0=ALU.max, op1=ALU.mult),
                      reads=[Bgc, Bl1], writes=[B_aT])
            if b + 1 < NBLK:
                prep_x(b + 1)
            for r in range(4):
                y_, By = yo[yc % 2]
                yc += 1
                for n in range(2):
                    py, Bpy = self.psum()
                    for j in range(8):
                        self.mm(py, Bpy, aT[:, j, r * 128:(r + 1) * 128], w2[:, j, n * 512:(n + 1) * 512], j == 0, j == 7, [B_aT, Bw2])
                    sc.op("dve", lambda h, y_=y_, py=py, n=n: h.tensor_tensor(out=y_[:, n * 512:(n + 1) * 512], in0=py,
                                                                              in1=b2_[:, n * 512:(n + 1) * 512], op=ALU.add),
                          reads=Bpy + [Bb2], writes=[By])
                r0 = b * BLK + r * 128
                sc.dma("sp", self.ys_d[r0:r0 + 128, :], y_, self.B_ys, By)
        sc.barrier()
        ar.release(m1)
        self.load_ln(self.lnp[l, 2], self.lnp[l, 3])
        y4 = [ar.alloc([128, 4, D], F32, "y4") for _ in range(2)]
        ht = [ar.alloc([128, D], F32, "cht") for _ in range(2)]
        zt = [ar.alloc([128, D], F32, "czt") for _ in range(2)]
        ot = [ar.alloc([128, D], F32, "cot") for _ in range(2)]
        stt = [ar.alloc([128, 16], F32, "cst") for _ in range(2)]
        dst_d, B_dst = (self.out, self.B_out) if last else (self.h_d, self.B_h)
        def gathers(tt):
            y_, By = y4[tt % 2]
            h_, Bh = ht[tt % 2]
            for k in range(4):
                sc.dma("pool", None, None, By, self.B_ys, fn=lambda h, y_=y_, tt=tt, k=k: h.indirect_dma_start(
                    out=y_[:, k, :], out_offset=None, in_=self.ys_d,
                    in_offset=bass.IndirectOffsetOnAxis(ap=idx4[:, tt, k:k + 1], axis=0)))
            sc.dma("sp", h_, self.h1_d[tt * 128:(tt + 1) * 128, :], Bh, self.B_h1)
        gathers(0)
        for tt in range(NTT):
            (y_, By), (h_, Bh), (z, Bz), (o, Bo), (st, Bst) = y4[tt % 2], ht[tt % 2], zt[tt % 2], ot[tt % 2], stt[tt % 2]
            if tt + 1 < NTT:
                gathers(tt + 1)
            sc.op("dve", lambda h, z=z, y_=y_, tt=tt: h.tensor_scalar(out=z, in0=y_[:, 0, :], scalar1=gate4[:, tt, 0:1], scalar2=None,
                                                                      op0=ALU.mult), reads=[By, B_g4], writes=[Bz])
            for k in range(1, 4):
                sc.op("dve", lambda h, z=z, y_=y_, tt=tt, k=k: h.scalar_tensor_tensor(out=z, in0=y_[:, k, :], scalar=gate4[:, tt, k:k + 1],
                                                                                     in1=z, op0=ALU.mult, op1=ALU.add),
                      reads=[By, B_g4, Bz], writes=[Bz])
            sc.op("dve", lambda h, z=z, h_=h_: h.scalar_tensor_tensor(out=z, in0=h_, scalar=DN_ALPHA, in1=z, op0=ALU.mult, op1=ALU.add),
                  reads=[Bh, Bz], writes=[Bz])
            self.ln_tile(z, Bz, o, Bo, st, Bst)
            sc.dma("sp", dst_d[tt * 128:(tt + 1) * 128, :], o, B_dst, Bo)
        sc.barrier()
        ar.release(m0)


def _kp(a, nk):
    L, R, N = a.shape
    return np.ascontiguousarray(a.reshape(L, nk, 128, N).transpose(0, 2, 1, 3))


def _pp(a, nt):
    L = a.shape[0]
    return np.ascontiguousarray(a.reshape(L, nt, 128).transpose(0, 2, 1))


def pack_shared(inp, depth, nexp=E):
    L = depth
    f = lambda k: np.asarray(inp[k], dtype=np.float32)[:L]
    d = {}
    d["ln_in"] = np.stack([np.asarray(inp["ln_in_g"], np.float32), np.asarray(inp["ln_in_b"], np.float32)])
    d["w_in"] = _kp(f("w_in"), 8)
    d["w_gate"] = _kp(f("w_gate"), 8)
    d["b_gate"] = _pp(f("b_gate"), 24)
    a = np.arange(128)[:, None, None]
    j = np.arange(5)[None, :, None]
    b = np.arange(128)[None, None, :]
    rel = (j - 4) * 128 + b - a
    qc = a // 64
    kc = (j - 4) * 2 + b // 64
    valid = (kc >= qc - 8) & (kc <= qc)
    idx = np.clip(rel, -128, 128) + 128
    rb = f("rel_bias")
    bq = rb[:, :, idx]
    bq = np.where(valid[None, None], bq, np.float32(NEG)).astype(np.float32)
    d["biasq"] = np.ascontiguousarray(bq.transpose(0, 2, 1, 3, 4)).reshape(L, 128, 8 * 5 * 128)
    cw = f("conv_w").transpose(0, 2, 1)
    d["conv_w"] = np.ascontiguousarray(cw.reshape(L, 4, 128, 31).transpose(0, 2, 1, 3)).reshape(L, 128, 124)
    d["conv_v"] = np.ascontiguousarray(np.concatenate([_pp(f("conv_b"), 4), _pp(f("conv_ln_g"), 4), _pp(f("conv_ln_b"), 4)], axis=2))
    d["w_pw2"] = _kp(f("w_pw2"), 4)
    d["qkv_g"] = np.ascontiguousarray(np.concatenate([_pp(f("q_norm_g"), 3), _pp(f("kv_norm_g"), 2)], axis=2))
    d["w_uq"] = _kp(f("w_uq"), 3)
    d["w_ukv"] = _kp(f("w_ukv"), 2)
    d["w_oa"] = _kp(f("w_oa"), 4)
    d["w_oc"] = _kp(f("w_oc"), 4)
    d["w_out"] = _kp(f("w_out"), 8)
    d["lnp"] = np.ascontiguousarray(np.stack([f("ln1_g"), f("ln1_b"), f("ln2_g"), f("ln2_b")], axis=1))
    d["w_router"] = _kp(f("w_router")[:, :, :nexp], 8).reshape(L, 128, 8 * nexp)
    d["b_router"] = np.ascontiguousarray(f("b_router")[:, :nexp])
    d["w1"] = np.ascontiguousarray(f("w1")[:, :nexp]).reshape(L * nexp * 1024, 2048)
    b1 = f("b1")[:, :nexp]
    d["b1"] = np.ascontiguousarray(b1.reshape(L, nexp, 16, 128).transpose(0, 1, 3, 2)).reshape(L * nexp * 128, 16)
    d["w2"] = np.ascontiguousarray(f("w2")[:, :nexp]).reshape(L * nexp * 1024, 1024)
    d["b2"] = np.ascontiguousarray(f("b2")[:, :nexp]).reshape(L * nexp, 1024)
    cv = np.zeros((128, 8), np.float32)
    inv = (10000.0 ** (-np.arange(0, 32, 2, dtype=np.float32) / 32)).astype(np.float32)
    cv[64:96, 0] = np.concatenate([inv, inv])
    cv[:, 1] = -math.pi
    cv[:, 6] = LN_EPS
    cv[:, 7] = RMS_EPS
    d["cvec"] = cv
    return d


def pack_core(inp, seqs):
    x = np.asarray(inp["x"], np.float32)[seqs].reshape(-1, D)
    pos = np.asarray(inp["positions"], np.int32)[seqs].reshape(-1)
    return {"x": np.ascontiguousarray(x), "posrep": np.ascontiguousarray(np.broadcast_to(pos[None, :], (32, pos.shape[0])))}


_CACHE = {}


def kernel(**inputs):
    n = 8
    nseq = 4
    if "nc" not in _CACHE:
        _CACHE["nc"] = Prog(nseq, 4).build()
    nc = _CACHE["nc"]
    shared = pack_shared(inputs, 4)
    in_maps = []
    for c in range(n):
        d = dict(shared)
        d.update(pack_core(inputs, list(range(c * nseq, (c + 1) * nseq))))
        in_maps.append(d)
    res = run_bass_kernel_spmd(nc, in_maps, core_ids=list(range(n)))
    out = np.concatenate([r["out"] for r in res.results], axis=0)
    return out.reshape(32, S, D).astype(np.float32)
```

```python
import contextlib
import math
import numpy as np
import concourse.bass as bass
import concourse.mybir as mybir
from concourse.bass_utils import run_bass_kernel_spmd

F32 = mybir.dt.float32
BF16 = mybir.dt.bfloat16
I32 = mybir.dt.int32
U32 = mybir.dt.uint32
AF = mybir.ActivationFunctionType
ALU = mybir.AluOpType
AX = mybir.AxisListType

D = 1024
S = 2048
NT = S // 128
NG = S // 512
IN_COLS = 3232
E = 32
BLK = 512
DN_ALPHA = 8.0 ** 0.25
LN_EPS = 1e-5
RMS_EPS = 1e-6
NEG = -30000.0
SW_ALPHA = 1.702
SW_LIM = 7.0


def _prod(xs):
    r = 1
    for x in xs:
        r *= int(x)
    return r


class Buf:
    __slots__ = ("name", "lw", "rd", "dsem", "dcnt", "excl")

    def __init__(self, name, excl=False):
        self.name = name
        self.excl = excl
        self.lw = None
        self.rd = {}
        self.dsem = None
        self.dcnt = 0


class Sched:
    def __init__(self, nc, stack):
        self.nc = nc
        self.stack = stack
        self.eng = {}
        for nm, h in (("pe", nc.tensor), ("act", nc.scalar), ("dve", nc.vector),
                      ("pool", nc.gpsimd), ("sp", nc.sync)):
            sem = stack.enter_context(nc.semaphore("e_" + nm))
            self.eng[nm] = dict(h=h, sem=sem, cnt=0, waited={})
        self.dbufs = []
        self.free_dsems = []
        self.ninst = 0

    def _wait(self, e, evs):
        best = {}
        for ev in evs:
            if ev is None:
                continue
            sem, val = ev
            k = id(sem)
            if k not in best or best[k][1] < val:
                best[k] = (sem, val)
        En = self.eng[e]
        for k, (sem, val) in best.items():
            if En["waited"].get(k, 0) < val:
                En["h"].wait_ge(sem, val)
                En["waited"][k] = val

    def op(self, e, fn, reads=(), writes=()):
        En = self.eng[e]
        xr = [b for b in reads if b.excl]
        if xr:
            reads = [b for b in reads if not b.excl]
            writes = list(writes) + xr
        evs = []
        for b in reads:
            evs.append(b.lw)
        for b in writes:
            evs.append(b.lw)
            evs.extend(b.rd.values())
        if e == "pe":
            evs = [ev for ev in evs if ev is not None and ev[0] is not En["sem"]]
        self._wait(e, evs)
        ins = fn(En["h"])
        En["cnt"] += 1
        ins.then_inc(En["sem"], 1)
        ev = (En["sem"], En["cnt"])
        for b in writes:
            b.lw = ev
            b.rd = {}
        for b in reads:
            b.rd[id(En["sem"])] = ev
        self.ninst += 1
        return ins

    def _dsem(self, b):
        if b.dsem is None:
            if self.free_dsems:
                b.dsem, b.dcnt = self.free_dsems.pop()
            else:
                b.dsem = self.stack.enter_context(self.nc.semaphore("d%d" % len(self.dbufs) + b.name))
                b.dcnt = 0
            self.dbufs.append(b)
        return b.dsem

    def dma(self, q, out, in_, dst, src=None, fn=None, **kw):
        En = self.eng[q]
        sem = self._dsem(dst)
        evs = []
        if src is not None:
            evs.append(src.lw)
        if dst.lw is not None and dst.lw[0] is not sem:
            evs.append(dst.lw)
        evs.extend(dst.rd.values())
        self._wait(q, evs)
        if fn is None:
            ins = En["h"].dma_start(out=out, in_=in_, **kw)
        else:
            ins = fn(En["h"])
        dst.dcnt += 16
        ins.then_inc(sem, 16)
        ev = (sem, dst.dcnt)
        dst.lw = ev
        dst.rd = {}
        if src is not None:
            src.rd[id(sem)] = ev
        self.ninst += 1
        return ins

    def barrier(self):
        evs = [(En["sem"], En["cnt"]) for En in self.eng.values() if En["cnt"] > 0]
        evs += [(b.dsem, b.dcnt) for b in self.dbufs if b.dcnt > 0]
        evs += [ev for ev in self.free_dsems if ev[1] > 0]
        for e in self.eng:
            self._wait(e, evs)
        for b in self.dbufs:
            self.free_dsems.append((b.dsem, b.dcnt))
            b.dsem = None
            b.lw = None
            b.rd = {}
        self.dbufs = []


class Arena:
    def __init__(self, t, n32):
        self.t = t
        self.n = n32
        self.off = 0
        self.cnt = 0

    def alloc(self, shape, dtype, name=None):
        sz = 2 if dtype == BF16 else 4
        nel = _prod(shape[1:])
        n32 = (nel * sz + 3) // 4
        assert self.off + n32 <= self.n, ("arena overflow", name, self.off, n32, self.n)
        v = self.t[0:shape[0], self.off:self.off + n32]
        self.off += n32
        if dtype != F32:
            v = v.bitcast(dtype)
        if nel != v.shape[1]:
            v = v[:, 0:nel]
        if len(shape) == 3:
            v = v.rearrange("p (a b) -> p a b", a=shape[1])
        elif len(shape) == 4:
            v = v.rearrange("p (a b c) -> p a b c", a=shape[1], b=shape[2])
        self.cnt += 1
        return v, Buf("%s%d" % (name or "t", self.cnt))

    def mark(self):
        return self.off

    def release(self, m):
        self.off = m


class Prog:
    def __init__(self, nseq, depth, dbg=False, stop=None, nexp=E):
        self.stop = stop
        self.E = nexp
        self.nseq = nseq
        self.depth = depth
        self.T = nseq * S
        self.NTT = self.T // 128
        self.NBLK = (4 * self.T + E * (BLK - 1) + BLK - 1) // BLK
        self.NROWS = self.NBLK * BLK
        self.dbg = dbg

    def build(self):
        nc = bass.Bass("TRN2", target_bir_lowering=False)
        self.nc = nc
        T, L = self.T, max(self.depth, 1)
        dt = nc.dram_tensor
        self.x = dt("x", [T, D], F32, kind="ExternalInput").ap()
        self.posrep = dt("posrep", [32, T], I32, kind="ExternalInput").ap()
        self.cvec = dt("cvec", [128, 8], F32, kind="ExternalInput").ap()
        self.ln_in = dt("ln_in", [2, D], F32, kind="ExternalInput").ap()
        self.w_in = dt("w_in", [L, 128, 8, IN_COLS], F32, kind="ExternalInput").ap()
        self.w_gate = dt("w_gate", [L, 128, 8, 3 * D], F32, kind="ExternalInput").ap()
        self.b_gate = dt("b_gate", [L, 128, 24], F32, kind="ExternalInput").ap()
        self.biasq = dt("biasq", [L, 128, 8 * 5 * 128], F32, kind="ExternalInput").ap()
        self.conv_w = dt("conv_w", [L, 128, 4 * 31], F32, kind="ExternalInput").ap()
        self.conv_v = dt("conv_v", [L, 128, 12], F32, kind="ExternalInput").ap()
        self.w_pw2 = dt("w_pw2", [L, 128, 4, D], F32, kind="ExternalInput").ap()
        self.qkv_g = dt("qkv_g", [L, 128, 5], F32, kind="ExternalInput").ap()
        self.w_uq = dt("w_uq", [L, 128, 3, 768], F32, kind="ExternalInput").ap()
        self.w_ukv = dt("w_ukv", [L, 128, 2, 1024], F32, kind="ExternalInput").ap()
        self.w_oa = dt("w_oa", [L, 128, 4, D], F32, kind="ExternalInput").ap()
        self.w_oc = dt("w_oc", [L, 128, 4, D], F32, kind="ExternalInput").ap()
        self.w_out = dt("w_out", [L, 128, 8, D], F32, kind="ExternalInput").ap()
        self.lnp = dt("lnp", [L, 4, D], F32, kind="ExternalInput").ap()
        self.w_router = dt("w_router", [L, 128, 8 * self.E], F32, kind="ExternalInput").ap()
        self.b_router = dt("b_router", [L, self.E], F32, kind="ExternalInput").ap()
        self.w1 = dt("w1", [L * self.E * 1024, 2048], F32, kind="ExternalInput").ap()
        self.b1 = dt("b1", [L * self.E * 128, 16], F32, kind="ExternalInput").ap()
        self.w2 = dt("w2", [L * self.E * 1024, 1024], F32, kind="ExternalInput").ap()
        self.b2 = dt("b2", [L * self.E, 1024], F32, kind="ExternalInput").ap()
        self.out = dt("out", [T, D], F32, kind="ExternalOutput").ap()
        self.h_d = dt("h_d", [T, D], F32, kind="Internal").ap()
        self.h1_d = dt("h1_d", [T, D], F32, kind="Internal").ap()
        self.h1b_d = dt("h1b_d", [T, D], BF16, kind="Internal").ap()
        self.xs_d = dt("xs_d", [self.NROWS, D], BF16, kind="Internal").ap()
        self.ys_d = dt("ys_d", [self.NROWS, D], F32, kind="Internal").ap()
        self.B_h = Buf("h_d")
        self.B_h1 = Buf("h1_d")
        self.B_h1b = Buf("h1b_d")
        self.B_xs = Buf("xs_d")
        self.B_ys = Buf("ys_d")
        self.B_out = Buf("out")
        self.rope_d = dt("rope_d", [2, 32, T], F32, kind="Internal").ap()
        self.B_rope = Buf("rope_d")
        self.aT_d = [dt("aT_d%d" % i, [128, 4, S], BF16, kind="Internal").ap() for i in range(3)]
        self.B_aT = [Buf("aT_d%d" % i) for i in range(3)]
        if self.dbg:
            self.dbg_h = dt("dbg_h", [T, D], F32, kind="ExternalOutput").ap()
            self.dbg_h1 = dt("dbg_h1", [T, D], F32, kind="ExternalOutput").ap()
            self.B_dbg = Buf("dbg")

        with contextlib.ExitStack() as stack:
            self.sc = Sched(nc, stack)
            at = stack.enter_context(nc.sbuf_tensor("arena", [128, 49600], F32))
            self.ar = Arena(at, 49600)
            ct = stack.enter_context(nc.sbuf_tensor("consts", [128, 3400], F32))
            self.car = Arena(ct, 3400)
            self.ps = stack.enter_context(nc.psum_tensor("ps", [128, 8 * 512], F32))
            self.psb = [Buf("ps%d" % i, excl=True) for i in range(8)]
            self.psi = 0
            self.consts()
            self.rope_tables()
            self.ln0()
            for l in range(self.depth):
                self.layer(l)
            if self.depth == 0:
                self.sc.dma("sp", self.out, self.h_d, self.B_out, self.B_h)
            elif self.stop == "mix":
                self.sc.dma("sp", self.out, self.h1_d, self.B_out, self.B_h1)
            elif self.stop is not None:
                self.sc.dma("sp", self.out, self.h_d, self.B_out, self.B_h)
            self.sc.barrier()
        return nc

    def dump(self, name, ap, B):
        o = self.nc.dram_tensor("dbg_" + name, list(ap.shape), ap.dtype, kind="ExternalOutput").ap()
        self.sc.dma("sp", o, ap, Buf("dbg_" + name), B)

    def psum(self, dtype=F32, banks=1, hi=8, fixed=None):
        if fixed is not None:
            i = fixed
        else:
            if self.psi + banks > hi:
                self.psi = 0
            i = self.psi
            self.psi = (self.psi + banks) % hi
        v = self.ps[:, i * 512:(i + banks) * 512]
        if dtype != F32:
            v = v.bitcast(dtype)
        return v, self.psb[i:i + banks]

    def consts(self):
        sc, nc = self.sc, self.nc
        car = self.car
        self.ident_f, self.B_c = car.alloc([128, 128], F32, "identf")
        B = self.B_c
        self.ident_b, _ = car.alloc([128, 128], BF16, "identb")
        self.ones_f, _ = car.alloc([128, 128], F32, "onesf")
        self.ones_b, _ = car.alloc([128, 128], BF16, "onesb")
        self.upper_b, _ = car.alloc([128, 128], BF16, "upper")
        self.cv, _ = car.alloc([128, 8], F32, "cvec")
        self.iota_e, _ = car.alloc([128, E], F32, "iotae")
        self.iota_p, _ = car.alloc([128, 1], F32, "iotap")
        self.lng, self.B_ln = car.alloc([128, D], F32, "lng")
        self.lnb, _ = car.alloc([128, D], F32, "lnb")
        tmpi, _ = car.alloc([128, 128], I32, "tmpi")
        sc.op("pool", lambda h: h.memset(self.ones_f, 1.0), writes=[B])
        sc.op("pool", lambda h: h.memset(self.ones_b, 1.0), writes=[B])
        sc.op("pool", lambda h: h.affine_select(out=self.ident_f, in_=self.ones_f, pattern=[[-1, 128]],
                                                 compare_op=ALU.is_equal, fill=0.0, base=0, channel_multiplier=1),
              reads=[B], writes=[B])
        sc.op("pool", lambda h: h.tensor_copy(out=self.ident_b, in_=self.ident_f), reads=[B], writes=[B])
        sc.op("pool", lambda h: h.affine_select(out=self.upper_b, in_=self.ones_b, pattern=[[1, 128]],
                                                 compare_op=ALU.is_gt, fill=0.0, base=0, channel_multiplier=-1),
              reads=[B], writes=[B])
        sc.op("pool", lambda h: h.iota(tmpi[:, 0:E], pattern=[[1, E]], base=0, channel_multiplier=0), writes=[B])
        sc.op("pool", lambda h: h.tensor_copy(out=self.iota_e, in_=tmpi[:, 0:E]), reads=[B], writes=[B])
        sc.op("pool", lambda h: h.iota(tmpi[:, 0:1], pattern=[[1, 1]], base=0, channel_multiplier=1), writes=[B])
        sc.op("pool", lambda h: h.tensor_copy(out=self.iota_p, in_=tmpi[:, 0:1]), reads=[B], writes=[B])
        sc.dma("sp", self.cv, self.cvec, B)

    def load_ln(self, g_ap, b_ap):
        sc = self.sc
        sc.dma("sp", self.lng, g_ap.partition_broadcast(128), self.B_ln)
        sc.dma("sp", self.lnb, b_ap.partition_broadcast(128), self.B_ln)

    def rsqrt(self, o, Bo, i, Bi, eps, scale):
        sc = self.sc
        sc.op("act", lambda h: h.activation(out=o, in_=i, func=AF.Sqrt, bias=self.eps_ap(eps, o.shape[0]), scale=scale),
              reads=[Bi, self.B_c], writes=[Bo])
        sc.op("dve", lambda h: h.reciprocal(out=o, in_=o), reads=[Bo], writes=[Bo])

    def eps_ap(self, eps, npart):
        i = {LN_EPS: 6, RMS_EPS: 7}[eps]
        return self.cv[0:npart, i:i + 1]

    def ln_tile(self, z, Bz, o, Bo, st, Bst):
        sc = self.sc
        stats = st[:, 0:12].rearrange("p (a b) -> p a b", a=2)
        for c in range(2):
            sc.op("dve", lambda h, c=c: h.bn_stats(out=stats[:, c, :], in_=z[:, c * 512:(c + 1) * 512]),
                  reads=[Bz], writes=[Bst])
        mv = st[:, 12:14]
        sc.op("dve", lambda h: h.bn_aggr(out=mv, in_=stats), reads=[Bst], writes=[Bst])
        rstd = st[:, 14:15]
        self.rsqrt(rstd, Bst, mv[:, 1:2], Bst, LN_EPS, 1.0)
        sc.op("dve", lambda h: h.tensor_scalar(out=o, in0=z, scalar1=mv[:, 0:1], scalar2=rstd,
                                               op0=ALU.subtract, op1=ALU.mult), reads=[Bz, Bst], writes=[Bo])
        sc.op("pool", lambda h: h.tensor_tensor(out=o, in0=o, in1=self.lng, op=ALU.mult),
              reads=[Bo, self.B_ln], writes=[Bo])
        sc.op("pool", lambda h: h.tensor_tensor(out=o, in0=o, in1=self.lnb, op=ALU.add),
              reads=[Bo, self.B_ln], writes=[Bo])

    def ln0(self):
        sc, ar = self.sc, self.ar
        m = ar.mark()
        self.load_ln(self.ln_in[0], self.ln_in[1])
        xt = [ar.alloc([128, D], F32, "x") for _ in range(2)]
        ot = [ar.alloc([128, D], F32, "o") for _ in range(2)]
        st = [ar.alloc([128, 16], F32, "st") for _ in range(2)]
        for t in range(self.NTT):
            (z, Bz), (o, Bo), (s_, Bs) = xt[t % 2], ot[t % 2], st[t % 2]
            sc.dma("sp", z, self.x[t * 128:(t + 1) * 128, :], Bz)
            self.ln_tile(z, Bz, o, Bo, s_, Bs)
            sc.dma("sp", self.h_d[t * 128:(t + 1) * 128, :], o, self.B_h, Bo)
        sc.barrier()
        ar.release(m)


    def mm(self, ps, Bps, lhsT, rhs, start, stop, reads):
        self.sc.op("pe", lambda h: h.matmul(ps, lhsT=lhsT, rhs=rhs, start=start, stop=stop),
                   reads=reads, writes=Bps)

    def wload(self, q, dst, Bd, src):
        n = dst.shape[-1]
        if dst.dtype == F32:
            self.sc.dma("sp", dst, src, Bd)
            return
        for c0 in range(0, n, 2048):
            c1 = min(n, c0 + 2048)
            if len(dst.shape) == 3:
                self.sc.dma("pool", dst[:, :, c0:c1], src[:, :, c0:c1], Bd)
            else:
                self.sc.dma("pool", dst[:, c0:c1], src[:, c0:c1], Bd)

    def rope_tables(self):
        sc, ar, nc = self.sc, self.ar, self.nc
        m = ar.mark()
        T = self.T
        CH = 2048
        for c0 in range(0, T, CH):
            pi_, Bp = ar.alloc([96, CH], I32, "posi")
            ang, Ba = ar.alloc([96, CH], F32, "ang")
            r, Br = ar.alloc([96, CH], F32, "r")
            o, Bo = ar.alloc([96, CH], F32, "o")
            sc.dma("sp", pi_[64:96, :], self.posrep[:, c0:c0 + CH], Bp)
            sc.op("dve", lambda h: h.tensor_copy(out=ang[64:96, :], in_=pi_[64:96, :]), reads=[Bp], writes=[Ba])
            sc.op("dve", lambda h: h.tensor_scalar(out=ang[64:96, :], in0=ang[64:96, :], scalar1=self.cv[64:96, 0:1],
                                                   scalar2=None, op0=ALU.mult), reads=[Ba, self.B_c], writes=[Ba])
            ki, Bk = ar.alloc([96, CH], I32, "ki")
            kf, Bkf = ar.alloc([96, CH], F32, "kf")
            cc, Bcc = ar.alloc([96, CH], F32, "cc")
            P_ = slice(64, 96)
            TWO_PI = 2 * math.pi
            PI_LO = 3.1415925
            for j, sh in enumerate((0.5 * math.pi, 0.0)):
                sc.op("dve", lambda h, sh=sh: h.tensor_scalar(out=r[P_, :], in0=ang[P_, :], scalar1=sh, scalar2=None, op0=ALU.add),
                      reads=[Ba], writes=[Br])
                sc.op("dve", lambda h: h.tensor_scalar(out=ki[P_, :], in0=r[P_, :], scalar1=1.0 / TWO_PI, scalar2=None, op0=ALU.mult),
                      reads=[Br], writes=[Bk])
                sc.op("dve", lambda h: h.tensor_copy(out=kf[P_, :], in_=ki[P_, :]), reads=[Bk], writes=[Bkf])
                sc.op("dve", lambda h: h.scalar_tensor_tensor(out=r[P_, :], in0=kf[P_, :], scalar=-TWO_PI, in1=r[P_, :],
                                                              op0=ALU.mult, op1=ALU.add), reads=[Bkf, Br], writes=[Br])
                sc.op("dve", lambda h: h.tensor_scalar(out=cc[P_, :], in0=r[P_, :], scalar1=math.pi, scalar2=-TWO_PI,
                                                       op0=ALU.is_gt, op1=ALU.mult), reads=[Br], writes=[Bcc])
                sc.op("dve", lambda h: h.tensor_tensor(out=r[P_, :], in0=r[P_, :], in1=cc[P_, :], op=ALU.add), reads=[Br, Bcc], writes=[Br])
                sc.op("dve", lambda h: h.tensor_scalar(out=cc[P_, :], in0=r[P_, :], scalar1=-math.pi, scalar2=TWO_PI,
                                                       op0=ALU.is_lt, op1=ALU.mult), reads=[Br], writes=[Bcc])
                sc.op("dve", lambda h: h.tensor_tensor(out=r[P_, :], in0=r[P_, :], in1=cc[P_, :], op=ALU.add), reads=[Br, Bcc], writes=[Br])
                sc.op("dve", lambda h: h.tensor_scalar(out=r[P_, :], in0=r[P_, :], scalar1=PI_LO, scalar2=-PI_LO,
                                                       op0=ALU.min, op1=ALU.max), reads=[Br], writes=[Br])
                sc.op("act", lambda h: h.activation(out=o[P_, :], in_=r[P_, :], func=AF.Sin), reads=[Br], writes=[Bo])
                sc.dma("sp", self.rope_d[j, :, c0:c0 + CH], o[64:96, :], self.B_rope, Bo)
            ar.release(m)
        sc.barrier()

    def layer(self, l):
        if self.stop == "rope":
            return
        for s in range(self.nseq):
            self.mixer_seq(l, s)
        self.sc.barrier()
        if self.stop is not None and self.stop != "route":
            return
        self.moe(l)

    def mixer_seq(self, l, s):
        sc, ar = self.sc, self.ar
        t0 = s * S
        m0 = ar.mark()
        hT, B_hT = ar.alloc([128, 8, S], BF16, "hT")
        mA = ar.mark()
        cqn, B_cq = ar.alloc([128, 3, S], BF16, "cqn")
        ckvn, B_ckv = ar.alloc([128, 2, S], BF16, "ckvn")
        krR, B_kr = ar.alloc([96, S], BF16, "krR")
        m_b3 = ar.mark()
        glu, B_glu = ar.alloc([128, 4, 30 + S], BF16, "glu")
        m_b2 = ar.mark()
        qkT, B_qk = ar.alloc([128, 8, S], BF16, "qkT")
        vA, B_vA = ar.alloc([128, NT, 8, 65], BF16, "vA")
        m_b1 = ar.mark()
        wbuf, B_w = ar.alloc([128, 8, 1696], BF16, "win")
        wkr, B_wkr = ar.alloc([128, 8, 96], BF16, "wkr")
        gq, B_gq = ar.alloc([128, 5], F32, "gq")
        mT = ar.mark()
        sc.dma("sp", gq, self.qkv_g[l], B_gq)
        sc.op("pool", lambda h: h.memset(glu[:, :, 0:30], 0.0), writes=[B_glu])
        sc.op("pool", lambda h: h.memset(vA[:, :, :, 64:65], 1.0), writes=[B_vA])
        hx = [ar.alloc([128, D], F32, "hx") for _ in range(2)]
        hb = [ar.alloc([128, D], BF16, "hb") for _ in range(2)]
        for tt in range(NT):
            (x_, Bx), (b_, Bb) = hx[tt % 2], hb[tt % 2]
            sc.dma("sp", x_, self.h_d[t0 + tt * 128:t0 + (tt + 1) * 128, :], Bx, self.B_h)
            sc.op("act", lambda h: h.copy(out=b_, in_=x_), reads=[Bx], writes=[Bb])
            ps, Bps = self.psum(BF16)
            for k in range(8):
                sc.op("pe", lambda h, k=k: h.transpose(ps[:, k * 128:(k + 1) * 128], b_[:, k * 128:(k + 1) * 128], self.ident_b),
                      reads=[Bb, self.B_c], writes=Bps)
            sc.op("dve", lambda h: h.tensor_copy(out=hT[:, :, tt * 128:(tt + 1) * 128],
                                                 in_=ps.rearrange("p (k n) -> p k n", k=8)),
                  reads=Bps, writes=[B_hT])
        ar.release(mT)
        if self.stop == "A0":
            sc.barrier()
            return
        tmp = [ar.alloc([128, 512], F32, "tmp") for _ in range(8)]
        tb = [ar.alloc([128, 512], BF16, "tb") for _ in range(2)]
        rc, B_rc = ar.alloc([96, 512], F32, "ropec")
        rs, B_rs = ar.alloc([96, 512], F32, "ropes")
        ti = [0]

        def T_():
            ti[0] += 1
            return tmp[ti[0] % 8]

        def fm_proj(col0, M, g, wb=wbuf, Bw=B_w):
            ps, Bps = self.psum()
            for k in range(8):
                self.mm(ps[0:M, :], Bps, wb[:, k, col0:col0 + M], hT[:, k, g * 512:(g + 1) * 512],
                        k == 0, k == 7, [Bw, B_hT])
            return ps, Bps

        self.wload("pool", wbuf[:, :, 0:1536], B_w, self.w_in[l, :, :, 0:1536])
        for g in range(NG):
            gs = slice(g * 512, (g + 1) * 512)
            for c in range(8):
                ps, Bps = fm_proj(c * 128, 128, g)
                if c < 4:
                    sc.op("act", lambda h, ps=ps, c=c: h.activation(out=qkT[:, c, gs], in_=ps, func=AF.Copy, scale=0.125),
                          reads=Bps, writes=[B_qk])
                else:
                    sc.op("dve", lambda h, ps=ps, c=c: h.tensor_copy(out=qkT[:, c, gs], in_=ps), reads=Bps, writes=[B_qk])
        for tt in range(NT):
            ps, Bps = self.psum()
            for k in range(8):
                self.mm(ps, Bps, hT[:, k, tt * 128:(tt + 1) * 128], wbuf[:, k, 1024:1536], k == 0, k == 7, [B_w, B_hT])
            sc.op("act" if tt % 2 else "dve",
                  (lambda h, ps=ps, tt=tt: h.copy(out=vA[:, tt, :, 0:64], in_=ps.rearrange("p (a b) -> p a b", a=8))) if tt % 2 else
                  (lambda h, ps=ps, tt=tt: h.tensor_copy(out=vA[:, tt, :, 0:64], in_=ps.rearrange("p (a b) -> p a b", a=8))),
                  reads=Bps, writes=[B_vA])
        if self.stop == "A1":
            sc.barrier()
            return
        self.wload("pool", wbuf[:, :, 0:1696], B_w, self.w_in[l, :, :, 1536:3232])
        sc.op("pool", lambda h: h.tensor_copy(out=wkr[:, :, 0:64], in_=wbuf[:, :, 1600:1664]), reads=[B_w], writes=[B_wkr])
        sc.op("pool", lambda h: h.tensor_scalar(out=wkr[:, :, 64:80], in0=wbuf[:, :, 1680:1696], scalar1=-1.0, scalar2=None,
                                                op0=ALU.mult), reads=[B_w], writes=[B_wkr])
        sc.op("pool", lambda h: h.tensor_copy(out=wkr[:, :, 80:96], in_=wbuf[:, :, 1664:1680]), reads=[B_w], writes=[B_wkr])
        for g in range(NG):
            gs = slice(g * 512, (g + 1) * 512)
            for c in range(4):
                pa, Bpa = fm_proj(c * 128, 128, g)
                pg, Bpg = fm_proj(512 + c * 128, 128, g)
                sg, Bsg = T_()
                sc.op("act", lambda h, pg=pg, sg=sg: h.activation(out=sg, in_=pg, func=AF.Sigmoid), reads=Bpg, writes=[Bsg])
                sc.op("dve", lambda h, pa=pa, sg=sg, c=c: h.tensor_tensor(out=glu[:, c, 30 + g * 512:30 + (g + 1) * 512],
                                                                         in0=pa, in1=sg, op=ALU.mult),
                      reads=Bpa + [Bsg], writes=[B_glu])
            if self.stop == "A2":
                sc.barrier()
                return
            for (c0, nt_, gcol, dstT, Bdst, nfeat) in ((1024, 3, 0, cqn, B_cq, 384), (1408, 2, 3, ckvn, B_ckv, 256)):
                raws = []
                pss, Bpss = None, None
                for j in range(nt_):
                    ps, Bps = fm_proj(c0 + j * 128, 128, g)
                    raw, Braw = T_()
                    sq, Bsq = T_()
                    sq = sq.bitcast(BF16)[:, 0:512]
                    sc.op("dve", lambda h, ps=ps, raw=raw: h.tensor_copy(out=raw, in_=ps), reads=Bps, writes=[Braw])
                    sc.op("act", lambda h, ps=ps, sq=sq: h.activation(out=sq, in_=ps, func=AF.Square), reads=Bps, writes=[Bsq])
                    raws.append((raw, Braw, sq, Bsq))
                pss, Bpss = self.psum()
                for j in range(nt_):
                    self.mm(pss, Bpss, self.ones_b, raws[j][2], j == 0, j == nt_ - 1, [self.B_c, raws[j][3]])
                rb, Brb = T_()
                sc.op("act", lambda h, pss=pss, rb=rb, nfeat=nfeat: h.activation(out=rb, in_=pss, func=AF.Sqrt,
                                                                             bias=self.eps_ap(RMS_EPS, 128), scale=1.0 / nfeat),
                      reads=Bpss + [self.B_c], writes=[Brb])
                sc.op("dve", lambda h, rb=rb: h.reciprocal(out=rb, in_=rb), reads=[Brb], writes=[Brb])
                for j in range(nt_):
                    raw, Braw = raws[j][0], raws[j][1]
                    sc.op("dve", lambda h, raw=raw, j=j, rb=rb, dstT=dstT, gcol=gcol:
                          h.scalar_tensor_tensor(out=dstT[:, j, gs], in0=raw, scalar=gq[:, gcol + j:gcol + j + 1], in1=rb,
                                                 op0=ALU.mult, op1=ALU.mult),
                          reads=[Braw, Brb, B_gq], writes=[Bdst])
            if self.stop == "A3":
                sc.barrier()
                return
            sc.dma("sp", rc[64:96, :], self.rope_d[0, :, t0 + g * 512:t0 + (g + 1) * 512], B_rc, self.B_rope)
            sc.dma("sp", rs[64:96, :], self.rope_d[1, :, t0 + g * 512:t0 + (g + 1) * 512], B_rs, self.B_rope)
            pa, Bpa = fm_proj(1600, 96, g)
            pb, Bpb = fm_proj(0, 96, g, wb=wkr, Bw=B_wkr)
            t1, Bt1 = T_()
            t2, Bt2 = T_()
            sc.op("dve", lambda h, pa=pa, t1=t1: h.tensor_tensor(out=t1[64:96, :], in0=pa[64:96, :], in1=rc[64:96, :], op=ALU.mult),
                  reads=Bpa + [B_rc], writes=[Bt1])
            sc.op("dve", lambda h, pb=pb, t2=t2: h.tensor_tensor(out=t2[64:96, :], in0=pb[64:96, :], in1=rs[64:96, :], op=ALU.mult),
                  reads=Bpb + [B_rs], writes=[Bt2])
            sc.op("pool", lambda h, t1=t1, t2=t2: h.tensor_tensor(out=krR[64:96, gs], in0=t1[64:96, :], in1=t2[64:96, :], op=ALU.add),
                  reads=[Bt1, Bt2], writes=[B_kr])
        sc.barrier()
        ar.release(m_b1)
        if self.stop == "A":
            self.dump("qkT", qkT, B_qk)
            self.dump("vA", vA, B_vA)
            self.dump("glu", glu, B_glu)
            self.dump("cqn", cqn, B_cq)
            self.dump("ckvn", ckvn, B_ckv)
            self.dump("krR", krR[64:96, :], B_kr)
            self.dump("hT", hT, B_hT)
            sc.barrier()
            return
        self.attn_a(l, s, qkT, B_qk, vA, B_vA)
        sc.barrier()
        ar.release(m_b2)
        if self.stop == "B1":
            return
        self.conv_b(l, s, glu, B_glu)
        sc.barrier()
        ar.release(m_b3)
        if self.stop == "B2":
            return
        self.attn_c(l, s, cqn, B_cq, ckvn, B_ckv, krR, B_kr)
        sc.barrier()
        ar.release(mA)
        if self.stop == "B3":
            for i in range(3):
                self.dump("aT%d" % i, self.aT_d[i], self.B_aT[i])
            sc.barrier()
            return
        self.mix_c(l, s, hT, B_hT)
        sc.barrier()
        ar.release(m0)

    def tr4(self, src, Bsrc, dst, Bdst, qi):
        sc = self.sc
        pt, Bpt = self.psum(BF16, hi=6)
        for j in range(4):
            sc.op("pe", lambda h, j=j: h.transpose(pt[:, j * 128:(j + 1) * 128], src[:, j * 128:(j + 1) * 128], self.ident_b),
                  reads=[Bsrc, self.B_c], writes=Bpt)
        sc.op("dve", lambda h: h.tensor_copy(out=dst[:, :, qi * 128:(qi + 1) * 128],
                                             in_=pt[:, 0:512].rearrange("p (k n) -> p k n", k=4)),
              reads=Bpt, writes=[Bdst])

    def attn_a(self, l, s, qkT, B_qk, vA, B_vA):
        sc, ar = self.sc, self.ar
        m = ar.mark()
        bq, B_bq = ar.alloc([128, 8, 5, 128], BF16, "bq")
        self.wload("pool", bq.rearrange("p a b c -> p (a b c)"), B_bq, self.biasq[l])
        Et = [ar.alloc([128, 5, 128], BF16, "E") for _ in range(2)]
        at = [ar.alloc([128, 512], BF16, "at") for _ in range(2)]
        rcp = [ar.alloc([128, 4], F32, "rcp") for _ in range(2)]
        aT, B_aT = ar.alloc([128, 4, S], BF16, "aTA")
        ec = 0
        for qi in range(NT):
            a_, Ba = at[qi % 2]
            kts = [kt for kt in range(qi - 4, qi + 1) if kt >= 0]
            nk = len(kts)
            for half in range(2):
                po, Bpo = self.psum(fixed=6 + half)
                pov = po[:, 0:260].rearrange("p (a b) -> p a b", a=4)
                for hh in range(4):
                    h_ = half * 4 + hh
                    tq = h_ // 2
                    pr = (h_ % 2) * 64
                    pS, BpS = self.psum(banks=2, hi=6)
                    for i, kt in enumerate(kts):
                        j = kt - qi + 4
                        o_ = pS[:, i * 128:(i + 1) * 128]
                        self.mm(o_, BpS, qkT[pr:pr + 64, 4 + tq, kt * 128:(kt + 1) * 128],
                                qkT[pr:pr + 64, tq, qi * 128:(qi + 1) * 128], True, False, [B_qk])
                        self.mm(o_, BpS, bq[:, h_, j, :], self.ident_b, False, True, [B_bq, self.B_c])
                    E_, BE = Et[ec % 2]
                    ec += 1
                    sc.op("act", lambda h, E_=E_, pS=pS, nk=nk: h.activation(
                        out=E_[:, 0:nk, :], in_=pS[:, 0:nk * 128].rearrange("p (a b) -> p a b", a=nk), func=AF.Exp),
                        reads=BpS, writes=[BE])
                    for i, kt in enumerate(kts):
                        self.mm(pov[:, hh, :], Bpo, E_[:, i, :], vA[:, kt, h_, :], i == 0, i == nk - 1, [BE, B_vA])
                r_, Br = rcp[half]
                sc.op("dve", lambda h, r_=r_, pov=pov: h.reciprocal(out=r_, in_=pov[:, :, 64]), reads=Bpo, writes=[Br])
                sc.op("dve", lambda h, r_=r_, pov=pov, half=half, a_=a_: h.tensor_tensor(
                    out=a_[:, half * 256:(half + 1) * 256].rearrange("p (a b) -> p a b", a=4), in0=pov[:, :, 0:64],
                    in1=r_.unsqueeze(2).to_broadcast([128, 4, 64]), op=ALU.mult), reads=Bpo + [Br], writes=[Ba])
            self.tr4(a_, Ba, aT, B_aT, qi)
        sc.dma("sp", self.aT_d[0], aT, self.B_aT[0], B_aT)
        sc.barrier()
        ar.release(m)

    def conv_b(self, l, s, glu, B_glu):
        sc, ar = self.sc, self.ar
        m = ar.mark()
        cw, B_cw = ar.alloc([128, 4, 31], F32, "cw")
        cvv, B_cv = ar.alloc([128, 12], F32, "cvv")
        sc.dma("sp", cw.rearrange("p a b -> p (a b)"), self.conv_w[l], B_cw)
        sc.dma("sp", cvv, self.conv_v[l], B_cv)
        xc = [ar.alloc([128, S], F32, "xc") for _ in range(4)]
        cT, B_cT = ar.alloc([128, 4, S], BF16, "cT")
        tmp = [ar.alloc([128, 512], F32, "ctmp") for _ in range(8)]
        ti = [0]

        def T_():
            ti[0] += 1
            return tmp[ti[0] % 8]
        dgs = [ar.alloc([128, 31, 128], BF16, "dg") for _ in range(4)]
        for ct in range(4):
            dg, Bdg = dgs[ct]
            for k in range(31):
                sc.op("dve", lambda h, dg=dg, ct=ct, k=k: h.tensor_scalar(out=dg[:, k, :], in0=self.ident_b, scalar1=cw[:, ct, k:k + 1],
                                                                         scalar2=None, op0=ALU.mult), reads=[self.B_c, B_cw], writes=[Bdg])
        for ct in range(4):
            dg, Bdg = dgs[ct]
            x_, Bx = xc[ct]
            for g in range(NG):
                ps, Bps = self.psum()
                for k in range(31):
                    self.mm(ps, Bps, dg[:, k, :], glu[:, ct, g * 512 + k:g * 512 + k + 512], k == 0, k == 30, [Bdg, B_glu])
                sc.op("act", lambda h, x_=x_, ps=ps, g=g, ct=ct: h.activation(out=x_[:, g * 512:(g + 1) * 512], in_=ps, func=AF.Identity,
                                                                             bias=cvv[:, ct:ct + 1], scale=1.0),
                      reads=Bps + [B_cv], writes=[Bx])
        for g in range(NG):
            gs = slice(g * 512, (g + 1) * 512)
            pm, Bpm = self.psum()
            pq, Bpq = self.psum()
            for ct in range(4):
                x_, Bx = xc[ct]
                sq, Bsq = T_()
                sc.op("act", lambda h, x_=x_, sq=sq: h.activation(out=sq, in_=x_[:, gs], func=AF.Square), reads=[Bx], writes=[Bsq])
                self.mm(pm, Bpm, self.ones_f, x_[:, gs], ct == 0, ct == 3, [self.B_c, Bx])
                self.mm(pq, Bpq, self.ones_f, sq, ct == 0, ct == 3, [self.B_c, Bsq])
            mean, Bm = T_()
            msq, Bq = T_()
            rstd, Br = T_()
            sc.op("act", lambda h: h.activation(out=mean, in_=pm, func=AF.Copy, scale=1.0 / 512), reads=Bpm, writes=[Bm])
            sc.op("dve", lambda h: h.tensor_tensor(out=msq, in0=mean, in1=mean, op=ALU.mult), reads=[Bm], writes=[Bq])
            sc.op("dve", lambda h: h.scalar_tensor_tensor(out=rstd, in0=pq, scalar=1.0 / 512, in1=msq, op0=ALU.mult, op1=ALU.subtract),
                  reads=Bpq + [Bq], writes=[Br])
            self.rsqrt(rstd, Br, rstd, Br, LN_EPS, 1.0)
            for ct in range(4):
                x_, Bx = xc[ct]
                d, Bd = T_()
                sc.op("dve", lambda h, x_=x_, d=d: h.tensor_tensor(out=d, in0=x_[:, gs], in1=mean, op=ALU.subtract),
                      reads=[Bx, Bm], writes=[Bd])
                sc.op("pool", lambda h, d=d: h.tensor_tensor(out=d, in0=d, in1=rstd, op=ALU.mult), reads=[Bd, Br], writes=[Bd])
                sc.op("act", lambda h, d=d, ct=ct: h.activation(out=cT[:, ct, gs], in_=d, func=AF.Silu,
                                                              bias=cvv[:, 8 + ct:9 + ct], scale=cvv[:, 4 + ct:5 + ct]),
                      reads=[Bd, B_cv], writes=[B_cT])
        sc.dma("sp", self.aT_d[1], cT, self.B_aT[1], B_cT)
        sc.barrier()
        ar.release(m)

    def attn_c(self, l, s, cqn, B_cq, ckvn, B_ckv, krR, B_kr):
        sc, ar = self.sc, self.ar
        t0 = s * S
        m = ar.mark()
        wq, B_wq = ar.alloc([128, 3, 768], BF16, "wq")
        wr, B_wr = ar.alloc([128, 3, 8, 96], BF16, "wr")
        wkv, B_wkv = ar.alloc([128, 2, 1024], BF16, "wkv")
        self.wload("pool", wq, B_wq, self.w_uq[l])
        self.wload("pool", wkv, B_wkv, self.w_ukv[l])
        wq4 = wq.rearrange("p k (a b) -> p k a b", a=8)
        for k in range(3):
            sc.op("pool", lambda h, k=k: h.tensor_copy(out=wr[:, k, :, 0:64], in_=wq4[:, k, :, 0:64]), reads=[B_wq], writes=[B_wr])
            sc.op("pool", lambda h, k=k: h.tensor_scalar(out=wr[:, k, :, 64:80], in0=wq4[:, k, :, 80:96], scalar1=-1.0, scalar2=None,
                                                        op0=ALU.mult), reads=[B_wq], writes=[B_wr])
            sc.op("pool", lambda h, k=k: h.tensor_copy(out=wr[:, k, :, 80:96], in_=wq4[:, k, :, 64:80]), reads=[B_wq], writes=[B_wr])
        rc, B_rc = ar.alloc([96, S], F32, "rc")
        rs, B_rs = ar.alloc([96, S], F32, "rs")
        sc.dma("sp", rc[64:96, :], self.rope_d[0, :, t0:t0 + S], B_rc, self.B_rope)
        sc.dma("sp", rs[64:96, :], self.rope_d[1, :, t0:t0 + S], B_rs, self.B_rope)
        Qh = [ar.alloc([96, S], BF16, "Qh") for _ in range(2)]
        Kh = [ar.alloc([96, S], BF16, "Kh") for _ in range(2)]
        Vh = [ar.alloc([128, NT, 65], BF16, "Vh") for _ in range(2)]
        Es = [ar.alloc([128, 16, 512], BF16, "Ec") for _ in range(2)]
        at, B_at = ar.alloc([128, NT, 512], BF16, "atC")
        aT, B_aT = ar.alloc([128, 4, S], BF16, "aTC")
        tmp = [ar.alloc([96, 512], F32, "mtmp") for _ in range(4)]
        rcp = [ar.alloc([128, 1], F32, "rcpc") for _ in range(4)]
        for i in range(2):
            sc.op("pool", lambda h, i=i: h.memset(Vh[i][0][:, :, 64:65], 1.0), writes=[Vh[i][1]])
        scale = 96.0 ** -0.5
        ec = 0
        tc_ = 0
        rcnt = 0
        for h_ in range(8):
            Q_, BQ = Qh[h_ % 2]
            K_, BK = Kh[h_ % 2]
            V_, BV = Vh[h_ % 2]
            for g in range(NG):
                gs = slice(g * 512, (g + 1) * 512)
                pa, Bpa = self.psum()
                pb, Bpb = self.psum()
                for k in range(3):
                    self.mm(pa[0:96, :], Bpa, wq[:, k, h_ * 96:(h_ + 1) * 96], cqn[:, k, gs], k == 0, k == 2, [B_wq, B_cq])
                for k in range(3):
                    self.mm(pb[0:96, :], Bpb, wr[:, k, h_, :], cqn[:, k, gs], k == 0, k == 2, [B_wr, B_cq])
                sc.op("act", lambda h, pa=pa, Q_=Q_: h.copy(out=Q_[0:64, gs], in_=pa[0:64, :]), reads=Bpa, writes=[BQ])
                t1, Bt1 = tmp[tc_ % 4]
                t2, Bt2 = tmp[(tc_ + 1) % 4]
                tc_ += 2
                sc.op("dve", lambda h, pa=pa, t1=t1: h.tensor_tensor(out=t1[64:96, :], in0=pa[64:96, :], in1=rc[64:96, gs], op=ALU.mult),
                      reads=Bpa + [B_rc], writes=[Bt1])
                sc.op("dve", lambda h, pb=pb, t2=t2: h.tensor_tensor(out=t2[64:96, :], in0=pb[64:96, :], in1=rs[64:96, gs], op=ALU.mult),
                      reads=Bpb + [B_rs], writes=[Bt2])
                sc.op("pool", lambda h, t1=t1, t2=t2, Q_=Q_: h.tensor_tensor(out=Q_[64:96, gs], in0=t1[64:96, :], in1=t2[64:96, :], op=ALU.add),
                      reads=[Bt1, Bt2], writes=[BQ])
                pk, Bpk = self.psum()
                for k in range(2):
                    self.mm(pk[0:64, :], Bpk, wkv[:, k, h_ * 128:h_ * 128 + 64], ckvn[:, k, gs], k == 0, k == 1, [B_wkv, B_ckv])
                sc.op("dve", lambda h, pk=pk, K_=K_: h.tensor_copy(out=K_[0:64, gs], in_=pk[0:64, :]), reads=Bpk, writes=[BK])
            sc.op("pool", lambda h, K_=K_: h.tensor_copy(out=K_[64:96, :], in_=krR[64:96, :]), reads=[B_kr], writes=[BK])
            for t8 in range(NT // 8):
                pv, Bpv = self.psum()
                for ti_ in range(8):
                    tt = t8 * 8 + ti_
                    for k in range(2):
                        self.mm(pv[:, ti_ * 64:(ti_ + 1) * 64], Bpv, ckvn[:, k, tt * 128:(tt + 1) * 128],
                                wkv[:, k, h_ * 128 + 64:h_ * 128 + 128], k == 0, k == 1, [B_wkv, B_ckv])
                sc.op("act", lambda h, pv=pv, V_=V_, t8=t8: h.copy(out=V_[:, t8 * 8:(t8 + 1) * 8, 0:64],
                                                                  in_=pv.rearrange("p (a b) -> p a b", a=8)),
                      reads=Bpv, writes=[BV])
            for G in range(NG):
                E_, BE = Es[ec % 2]
                ec += 1
                nkt = 4 * G + 4
                for kt in range(nkt):
                    pS, BpS = self.psum()
                    self.mm(pS, BpS, K_[0:96, kt * 128:(kt + 1) * 128], Q_[0:96, G * 512:(G + 1) * 512], True, True, [BK, BQ])
                    sc.op("act", lambda h, pS=pS, E_=E_, kt=kt: h.activation(out=E_[:, kt, :], in_=pS, func=AF.Exp, scale=scale),
                          reads=BpS, writes=[BE])
                    if kt >= 4 * G:
                        ql = kt - 4 * G
                        sc.op("pool", lambda h, E_=E_, kt=kt, ql=ql: h.memset(E_[64:128, kt, ql * 128:ql * 128 + 64], 0.0), writes=[BE])
                for ql in range(4):
                    qi = 4 * G + ql
                    po, Bpo = self.psum()
                    for kt in range(qi + 1):
                        self.mm(po[:, 0:65], Bpo, E_[:, kt, ql * 128:(ql + 1) * 128], V_[:, kt, :], kt == 0, kt == qi, [BE, BV])
                    r_, Br = rcp[rcnt % 4]
                    rcnt += 1
                    sc.op("dve", lambda h, r_=r_, po=po: h.reciprocal(out=r_, in_=po[:, 64:65]), reads=Bpo, writes=[Br])
                    sc.op("dve", lambda h, r_=r_, po=po, qi=qi, h_=h_: h.tensor_scalar(
                        out=at[:, qi, h_ * 64:(h_ + 1) * 64], in0=po[:, 0:64], scalar1=r_, scalar2=None, op0=ALU.mult),
                        reads=Bpo + [Br], writes=[B_at])
        for qi in range(NT):
            self.tr4(at[:, qi, :], B_at, aT, B_aT, qi)
        sc.dma("sp", self.aT_d[2], aT, self.B_aT[2], B_aT)
        sc.barrier()
        ar.release(m)

    def mix_c(self, l, s, hT, B_hT):
        sc, ar = self.sc, self.ar
        t0 = s * S
        m = ar.mark()
        wgs = [ar.alloc([128, 8, D], BF16, "wg") for _ in range(3)]
        wo = [ar.alloc([128, 4, D], BF16, "wo") for _ in range(3)]
        wout, B_wout = ar.alloc([128, 8, D], BF16, "wout")
        bg, B_bg = ar.alloc([128, 24], F32, "bg")
        for br, src in enumerate((self.w_oa, self.w_pw2, self.w_oc)):
            self.wload("pool", wo[br][0], wo[br][1], src[l])
            self.wload("pool", wgs[br][0], wgs[br][1], self.w_gate[l, :, :, br * D:(br + 1) * D])
        self.wload("pool", wout, B_wout, self.w_out[l])
        sc.dma("sp", bg, self.b_gate[l], B_bg)
        self.load_ln(self.lnp[l, 0], self.lnp[l, 1])
        aTg = [ar.alloc([128, 4, 512], BF16, "aTg") for _ in range(3)]
        mT, B_mT = ar.alloc([128, 8, 512], BF16, "mT")
        macc = [ar.alloc([128, 512], F32, "macc") for _ in range(8)]
        sgs = [ar.alloc([128, 512], F32, "sg") for _ in range(2)]
        tms = [ar.alloc([128, 512], F32, "tm") for _ in range(2)]
        zt = [ar.alloc([128, D], F32, "zt") for _ in range(2)]
        ot = [ar.alloc([128, D], F32, "ot") for _ in range(2)]
        obt = [ar.alloc([128, D], BF16, "obt") for _ in range(2)]
        stt = [ar.alloc([128, 16], F32, "stt") for _ in range(2)]
        cnt = 0
        for g in range(NG):
            gs = slice(g * 512, (g + 1) * 512)
            for br in range(3):
                sc.dma("sp", aTg[br][0], self.aT_d[br][:, :, gs], aTg[br][1], self.B_aT[br])
            for br in range(3):
                wg, B_wg = wgs[br]
                for c in range(8):
                    ma, Bma = macc[c]
                    py, Bpy = self.psum()
                    for k in range(4):
                        self.mm(py, Bpy, wo[br][0][:, k, c * 128:(c + 1) * 128], aTg[br][0][:, k, :], k == 0, k == 3,
                                [wo[br][1], aTg[br][1]])
                    pg, Bpg = self.psum()
                    for k in range(8):
                        self.mm(pg, Bpg, wg[:, k, c * 128:(c + 1) * 128], hT[:, k, gs], k == 0, k == 7, [B_wg, B_hT])
                    sg, Bsg = sgs[cnt % 2]
                    tm, Btm = tms[cnt % 2]
                    cnt += 1
                    sc.op("act", lambda h, sg=sg, pg=pg, br=br, c=c: h.activation(out=sg, in_=pg, func=AF.Sigmoid,
                                                                               bias=bg[:, br * 8 + c:br * 8 + c + 1], scale=1.0),
                          reads=Bpg + [B_bg], writes=[Bsg])
                    if br == 0:
                        sc.op("dve", lambda h, ma=ma, py=py, sg=sg: h.tensor_tensor(out=ma, in0=py, in1=sg, op=ALU.mult),
                              reads=Bpy + [Bsg], writes=[Bma])
                    else:
                        sc.op("dve", lambda h, tm=tm, py=py, sg=sg: h.tensor_tensor(out=tm, in0=py, in1=sg, op=ALU.mult),
                              reads=Bpy + [Bsg], writes=[Btm])
                        if br == 1:
                            sc.op("pool", lambda h, ma=ma, tm=tm: h.tensor_tensor(out=ma, in0=ma, in1=tm, op=ALU.add),
                                  reads=[Bma, Btm], writes=[Bma])
                        else:
                            sc.op("pool", lambda h, ma=ma, tm=tm, c=c: h.tensor_tensor(out=mT[:, c, :], in0=ma, in1=tm, op=ALU.add),
                                  reads=[Bma, Btm], writes=[B_mT])
            for ql in range(4):
                tt = g * 4 + ql
                r0 = t0 + tt * 128
                (z, Bz), (o, Bo), (ob, Bob), (st, Bst) = zt[tt % 2], ot[tt % 2], obt[tt % 2], stt[tt % 2]
                sc.dma("sp", z, self.h_d[r0:r0 + 128, :], Bz, self.B_h)
                for n in range(2):
                    pz, Bpz = self.psum()
                    for c in range(8):
                        self.mm(pz, Bpz, mT[:, c, ql * 128:(ql + 1) * 128], wout[:, c, n * 512:(n + 1) * 512], c == 0, c == 7,
                                [B_mT, B_wout])
                    sc.op("dve", lambda h, z=z, pz=pz, n=n: h.scalar_tensor_tensor(
                        out=z[:, n * 512:(n + 1) * 512], in0=z[:, n * 512:(n + 1) * 512], scalar=DN_ALPHA, in1=pz,
                        op0=ALU.mult, op1=ALU.add), reads=[Bz] + Bpz, writes=[Bz])
                self.ln_tile(z, Bz, o, Bo, st, Bst)
                sc.dma("sp", self.h1_d[r0:r0 + 128, :], o, self.B_h1, Bo)
                sc.op("act", lambda h, ob=ob, o=o: h.copy(out=ob, in_=o), reads=[Bo], writes=[Bob])
                sc.dma("sp", self.h1b_d[r0:r0 + 128, :], ob, self.B_h1b, Bob)
        sc.barrier()
        ar.release(m)

    def hs_scan(self, bufs, n, view):
        sc = self.sc
        cur = 0
        sft = 1
        while sft < n:
            (a, Ba), (b, Bb) = bufs[cur], bufs[1 - cur]
            sc.op("dve", lambda h, a=a, b=b, sft=sft: h.tensor_tensor(out=view(b, sft, n), in0=view(a, sft, n), in1=view(a, 0, n - sft),
                                                                     op=ALU.add), reads=[Ba], writes=[Bb])
            sc.op("dve", lambda h, a=a, b=b, sft=sft: h.tensor_copy(out=view(b, 0, sft), in_=view(a, 0, sft)), reads=[Ba], writes=[Bb])
            cur = 1 - cur
            sft *= 2
        return cur

    def moe(self, l):
        sc, ar, nc = self.sc, self.ar, self.nc
        E_, T, NTT, NBLK = self.E, self.T, self.NTT, self.NBLK
        last = (l == self.depth - 1)
        m0 = ar.mark()
        idx4, B_idx4 = ar.alloc([128, NTT, 4], U32, "idx4")
        gate4, B_g4 = ar.alloc([128, NTT, 4], F32, "gate4")
        idxw, B_idxw = ar.alloc([128, NBLK, 8], U32, "idxw")
        idxb1, B_ib1 = ar.alloc([128, NBLK], U32, "idxb1")
        idxb2, B_ib2 = ar.alloc([128, NBLK], U32, "idxb2")
        m1 = ar.mark()
        wr32, B_wr = ar.alloc([128, 8, E_], F32, "wr32")
        brt, B_brt = ar.alloc([128, E_], F32, "brt")
        sc.dma("sp", wr32.rearrange("p a b -> p (a b)"), self.w_router[l], B_wr)
        sc.dma("sp", brt, self.b_router[l].partition_broadcast(128), B_brt)
        mask, B_mask = ar.alloc([128, NTT, E_], F32, "mask")
        topv, B_topv = ar.alloc([128, NTT, 8], F32, "topv")
        eid, B_eid = ar.alloc([128, NTT, 8], U32, "eid")
        hx = [ar.alloc([128, D], F32, "mhx") for _ in range(2)]
        hT32 = [ar.alloc([128, 8, 128], F32, "hT32") for _ in range(2)]
        lgs = [ar.alloc([128, E_], F32, "lg") for _ in range(2)]
        for tt in range(NTT):
            (x_, Bx), (t_, Bt), (lg, Blg) = hx[tt % 2], hT32[tt % 2], lgs[tt % 2]
            sc.dma("sp", x_, self.h1_d[tt * 128:(tt + 1) * 128, :], Bx, self.B_h1)
            for hf in range(2):
                ps, Bps = self.psum()
                for k in range(4):
                    kk = hf * 4 + k
                    sc.op("pe", lambda h, ps=ps, k=k, kk=kk, x_=x_: h.transpose(ps[:, k * 128:(k + 1) * 128], x_[:, kk * 128:(kk + 1) * 128],
                                                                             self.ident_f), reads=[Bx, self.B_c], writes=Bps)
                sc.op("act" if hf else "dve",
                      (lambda h, ps=ps, t_=t_, hf=hf: h.copy(out=t_[:, hf * 4:(hf + 1) * 4, :], in_=ps.rearrange("p (a b) -> p a b", a=4))) if hf else
                      (lambda h, ps=ps, t_=t_, hf=hf: h.tensor_copy(out=t_[:, hf * 4:(hf + 1) * 4, :], in_=ps.rearrange("p (a b) -> p a b", a=4))),
                      reads=Bps, writes=[Bt])
            pl, Bpl = self.psum()
            for k in range(8):
                self.mm(pl[:, 0:E_], Bpl, t_[:, k, :], wr32[:, k, :], k == 0, k == 7, [Bt, B_wr])
            sc.op("dve", lambda h, lg=lg, pl=pl: h.tensor_tensor(out=lg, in0=pl[:, 0:E_], in1=brt, op=ALU.add), reads=Bpl + [B_brt], writes=[Blg])
            sc.op("dve", lambda h, lg=lg, tt=tt: h.max(out=topv[:, tt, :], in_=lg), reads=[Blg], writes=[B_topv])
            sc.op("dve", lambda h, lg=lg, tt=tt: h.max_index(out=eid[:, tt, :], in_max=topv[:, tt, :], in_values=lg),
                  reads=[Blg, B_topv], writes=[B_eid])
            sc.op("dve", lambda h, lg=lg, tt=tt: h.tensor_scalar(out=mask[:, tt, :], in0=lg, scalar1=topv[:, tt, 3:4], scalar2=None,
                                                                op0=ALU.is_ge), reads=[Blg, B_topv], writes=[B_mask])
        d4, B_d4 = ar.alloc([128, NTT, 4], F32, "d4")
        s4, B_s4 = ar.alloc([128, NTT], F32, "s4")
        sc.op("dve", lambda h: h.tensor_tensor(out=d4, in0=topv[:, :, 0:4], in1=topv[:, :, 0:1].to_broadcast([128, NTT, 4]), op=ALU.subtract),
              reads=[B_topv], writes=[B_d4])
        sc.op("act", lambda h: h.activation(out=d4, in_=d4, func=AF.Exp), reads=[B_d4], writes=[B_d4])
        sc.op("dve", lambda h: h.reduce_sum(out=s4, in_=d4, axis=AX.X), reads=[B_d4], writes=[B_s4])
        sc.op("dve", lambda h: h.reciprocal(out=s4, in_=s4), reads=[B_s4], writes=[B_s4])
        sc.op("dve", lambda h: h.tensor_tensor(out=gate4, in0=d4, in1=s4.unsqueeze(2).to_broadcast([128, NTT, 4]), op=ALU.mult),
              reads=[B_d4, B_s4], writes=[B_g4])
        NC_ = NTT * E_
        maskb, B_mb = ar.alloc([128, NC_], BF16, "maskb")
        pre, B_pre = ar.alloc([128, NTT, E_], F32, "pre")
        tot = [ar.alloc([128, NTT, E_], F32, "tot") for _ in range(2)]
        tot0, B_tot0 = ar.alloc([128, NTT, E_], F32, "tot0")
        mflat = mask.rearrange("p a b -> p (a b)")
        sc.op("dve", lambda h: h.tensor_copy(out=maskb, in_=mflat), reads=[B_mask], writes=[B_mb])
        for c0 in range(0, NC_, 512):
            c1 = min(NC_, c0 + 512)
            pp, Bpp = self.psum()
            pt_, Bpt = self.psum()
            self.mm(pp[:, 0:c1 - c0], Bpp, self.upper_b, maskb[:, c0:c1], True, True, [self.B_c, B_mb])
            self.mm(pt_[:, 0:c1 - c0], Bpt, self.ones_b, maskb[:, c0:c1], True, True, [self.B_c, B_mb])
            sc.op("dve", lambda h, pp=pp, c0=c0, c1=c1: h.tensor_copy(out=pre.rearrange("p a b -> p (a b)")[:, c0:c1], in_=pp[:, 0:c1 - c0]),
                  reads=Bpp, writes=[B_pre])
            sc.op("act", lambda h, pt_=pt_, c0=c0, c1=c1: h.copy(out=tot0.rearrange("p a b -> p (a b)")[:, c0:c1], in_=pt_[:, 0:c1 - c0]),
                  reads=Bpt, writes=[B_tot0])
        sc.op("dve", lambda h: h.tensor_copy(out=tot[0][0], in_=tot0), reads=[B_tot0], writes=[tot[0][1]])
        cur = self.hs_scan(tot, NTT, lambda a, lo, hi: a[:, lo:hi, :])
        inc, B_inc = tot[cur]
        cnt, B_cnt = ar.alloc([128, E_], F32, "cnt")
        sc.op("dve", lambda h: h.tensor_copy(out=cnt, in_=inc[:, NTT - 1, :]), reads=[B_inc], writes=[B_cnt])
        posd, B_pos = ar.alloc([128, NTT, E_], F32, "posd")
        sc.op("dve", lambda h: h.tensor_tensor(out=posd, in0=inc, in1=tot0, op=ALU.subtract), reads=[B_inc, B_tot0], writes=[B_pos])
        sc.op("dve", lambda h: h.tensor_tensor(out=posd, in0=posd, in1=pre, op=ALU.add), reads=[B_pos, B_pre], writes=[B_pos])
        J = T // BLK
        ij, B_ij = ar.alloc([128, J], I32, "ij")
        thr, B_thr = ar.alloc([128, J], F32, "thr")
        sc.op("pool", lambda h: h.iota(ij, pattern=[[1, J]], base=0, channel_multiplier=0), writes=[B_ij])
        sc.op("dve", lambda h: h.tensor_copy(out=thr, in_=ij), reads=[B_ij], writes=[B_thr])
        sc.op("dve", lambda h: h.tensor_scalar(out=thr, in0=thr, scalar1=float(BLK), scalar2=None, op0=ALU.mult), reads=[B_thr], writes=[B_thr])
        cmpj, B_cmpj = ar.alloc([128, E_, J], F32, "cmpj")
        sc.op("dve", lambda h: h.tensor_tensor(out=cmpj, in0=cnt.unsqueeze(2).to_broadcast([128, E_, J]),
                                               in1=thr.unsqueeze(1).to_broadcast([128, E_, J]), op=ALU.is_gt),
              reads=[B_cnt, B_thr], writes=[B_cmpj])
        pe_ = [ar.alloc([128, E_], F32, "pe") for _ in range(2)]
        nbk, B_nbk = ar.alloc([128, E_], F32, "nbk")
        sc.op("dve", lambda h: h.reduce_sum(out=nbk, in_=cmpj, axis=AX.X), reads=[B_cmpj], writes=[B_nbk])
        sc.op("dve", lambda h: h.tensor_scalar(out=nbk, in0=nbk, scalar1=float(BLK), scalar2=None, op0=ALU.mult), reads=[B_nbk], writes=[B_nbk])
        sc.op("dve", lambda h: h.tensor_copy(out=pe_[0][0], in_=nbk), reads=[B_nbk], writes=[pe_[0][1]])
        cur = self.hs_scan(pe_, E_, lambda a, lo, hi: a[:, lo:hi])
        pend, B_pend = pe_[cur]
        pst, B_pst = pe_[1 - cur]
        sc.op("dve", lambda h: h.tensor_tensor(out=pst, in0=pend, in1=nbk, op=ALU.subtract), reads=[B_pend, B_nbk], writes=[B_pst])
        sc.op("dve", lambda h: h.tensor_tensor(out=posd, in0=posd, in1=pst.unsqueeze(1).to_broadcast([128, NTT, E_]), op=ALU.add),
              reads=[B_pos, B_pst], writes=[B_pos])
        eidf, B_eidf = ar.alloc([128, NTT, 4], F32, "eidf")
        sc.op("dve", lambda h: h.tensor_copy(out=eidf, in_=eid[:, :, 0:4]), reads=[B_eid], writes=[B_eidf])
        eq, B_eq = ar.alloc([128, NTT, E_], F32, "eq")
        d4f, B_d4f = ar.alloc([128, NTT, 4], F32, "d4f")
        for k in range(4):
            sc.op("dve", lambda h, k=k: h.tensor_tensor(out=eq, in0=self.iota_e[:, 0:E_].unsqueeze(1).to_broadcast([128, NTT, E_]),
                                                        in1=eidf[:, :, k:k + 1].to_broadcast([128, NTT, E_]), op=ALU.is_equal),
                  reads=[self.B_c, B_eidf], writes=[B_eq])
            sc.op("dve", lambda h: h.tensor_tensor(out=eq, in0=eq, in1=posd, op=ALU.mult), reads=[B_eq, B_pos], writes=[B_eq])
            sc.op("dve", lambda h, k=k: h.reduce_sum(out=d4f[:, :, k], in_=eq, axis=AX.X), reads=[B_eq], writes=[B_d4f])
        sc.op("dve", lambda h: h.tensor_copy(out=idx4, in_=d4f), reads=[B_d4f], writes=[B_idx4])
        ib, B_ib = ar.alloc([128, NBLK], I32, "ib")
        bst, B_bst = ar.alloc([128, NBLK], F32, "bst")
        sc.op("pool", lambda h: h.iota(ib, pattern=[[1, NBLK]], base=0, channel_multiplier=0), writes=[B_ib])
        sc.op("dve", lambda h: h.tensor_copy(out=bst, in_=ib), reads=[B_ib], writes=[B_bst])
        sc.op("dve", lambda h: h.tensor_scalar(out=bst, in0=bst, scalar1=float(BLK), scalar2=None, op0=ALU.mult), reads=[B_bst], writes=[B_bst])
        cmpb, B_cmpb = ar.alloc([128, NBLK, E_], F32, "cmpb")
        sc.op("dve", lambda h: h.tensor_tensor(out=cmpb, in0=pend.unsqueeze(1).to_broadcast([128, NBLK, E_]),
                                               in1=bst.unsqueeze(2).to_broadcast([128, NBLK, E_]), op=ALU.is_le),
              reads=[B_pend, B_bst], writes=[B_cmpb])
        bex, B_bex = ar.alloc([128, NBLK], F32, "bex")
        sc.op("dve", lambda h: h.reduce_sum(out=bex, in_=cmpb, axis=AX.X), reads=[B_cmpb], writes=[B_bex])
        sc.op("dve", lambda h: h.tensor_scalar(out=bex, in0=bex, scalar1=float(E_ - 1), scalar2=float(l * E_), op0=ALU.min, op1=ALU.add),
              reads=[B_bex], writes=[B_bex])
        sc.op("dve", lambda h: h.tensor_copy(out=idxb2, in_=bex), reads=[B_bex], writes=[B_ib2])
        t1_, B_t1 = ar.alloc([128, NBLK], F32, "t1_")
        sc.op("dve", lambda h: h.tensor_scalar(out=t1_, in0=bex, scalar1=128.0, scalar2=self.iota_p[:, 0:1], op0=ALU.mult, op1=ALU.add),
              reads=[B_bex, self.B_c], writes=[B_t1])
        sc.op("dve", lambda h: h.tensor_copy(out=idxb1, in_=t1_), reads=[B_t1], writes=[B_ib1])
        sc.op("dve", lambda h: h.tensor_scalar(out=t1_, in0=bex, scalar1=1024.0, scalar2=self.iota_p[:, 0:1], op0=ALU.mult, op1=ALU.add),
              reads=[B_bex, self.B_c], writes=[B_t1])
        t8, B_t8 = ar.alloc([128, NBLK, 8], F32, "t8")
        for k in range(8):
            sc.op("dve", lambda h, k=k: h.tensor_scalar(out=t8[:, :, k], in0=t1_, scalar1=float(k * 128), scalar2=None, op0=ALU.add),
                  reads=[B_t1], writes=[B_t8])
        sc.op("dve", lambda h: h.tensor_copy(out=idxw, in_=t8), reads=[B_t8], writes=[B_idxw])
        if self.stop == "route":
            self.dump("idx4", idx4, B_idx4)
            self.dump("gate4", gate4, B_g4)
            self.dump("idxw", idxw, B_idxw)
            self.dump("cnt", cnt, B_cnt)
            sc.barrier()
            ar.release(m0)
            return
        sc.barrier()
        ar.release(m1)
        hb = [ar.alloc([128, D], BF16, "shb") for _ in range(3)]
        for tt in range(NTT):
            b_, Bb = hb[tt % 3]
            sc.dma("sp", b_, self.h1b_d[tt * 128:(tt + 1) * 128, :], Bb, self.B_h1b)
            for k in range(4):
                sc.dma("pool", None, None, self.B_xs, Bb,
                       fn=lambda h, b_=b_, tt=tt, k=k: h.indirect_dma_start(
                           out=self.xs_d, out_offset=bass.IndirectOffsetOnAxis(ap=idx4[:, tt, k:k + 1], axis=0),
                           in_=b_, in_offset=None))
        sc.barrier()
        ar.release(m1)
        W1 = [ar.alloc([128, 8, 2048], BF16, "W1") for _ in range(2)]
        W2 = [ar.alloc([128, 8, D], BF16, "W2") for _ in range(2)]
        b1t = [ar.alloc([128, 16], F32, "b1t") for _ in range(2)]
        b2t = [ar.alloc([128, D], F32, "b2t") for _ in range(2)]
        b1ps = [ar.alloc([128, 8], F32, "b1p") for _ in range(2)]
        xr = [ar.alloc([128, D], BF16, "xr") for _ in range(4)]
        xT = [ar.alloc([128, 8, BLK], BF16, "xT") for _ in range(2)]
        aT, B_aT = ar.alloc([128, 8, BLK], BF16, "aTm")
        tmp = [ar.alloc([128, 512], F32, "etmp") for _ in range(6)]
        yo = [ar.alloc([128, D], F32, "yo") for _ in range(2)]
        ti = [0]

        def T_():
            ti[0] += 1
            return tmp[ti[0] % 6]
        yc = 0

        def prep_x(b):
            xT_, BxT = xT[b % 2]
            for r in range(4):
                x_, Bx = xr[r]
                r0 = b * BLK + r * 128
                sc.dma("pool", x_, self.xs_d[r0:r0 + 128, :], Bx, self.B_xs)
                ps, Bps = self.psum(BF16)
                for k in range(8):
                    sc.op("pe", lambda h, ps=ps, k=k, x_=x_: h.transpose(ps[:, k * 128:(k + 1) * 128], x_[:, k * 128:(k + 1) * 128], self.ident_b),
                          reads=[Bx, self.B_c], writes=Bps)
                if r % 2:
                    sc.op("dve", lambda h, ps=ps, r=r: h.tensor_copy(out=xT_[:, :, r * 128:(r + 1) * 128], in_=ps.rearrange("p (k n) -> p k n", k=8)),
                          reads=Bps, writes=[BxT])
                else:
                    sc.op("act", lambda h, ps=ps, r=r: h.copy(out=xT_[:, :, r * 128:(r + 1) * 128], in_=ps.rearrange("p (k n) -> p k n", k=8)),
                          reads=Bps, writes=[BxT])
        for b in range(NBLK):
            (w1, Bw1), (w2, Bw2), (b1_, Bb1), (b2_, Bb2), (xT_, BxT) = W1[b % 2], W2[b % 2], b1t[b % 2], b2t[b % 2], xT[b % 2]
            for k in range(8):
                sc.dma("pool", None, None, Bw1, None, fn=lambda h, w1=w1, b=b, k=k: h.indirect_dma_start(
                    out=w1[:, k, :], out_offset=None, in_=self.w1,
                    in_offset=bass.IndirectOffsetOnAxis(ap=idxw[:, b, k:k + 1], axis=0)))
            for k in range(8):
                sc.dma("pool", None, None, Bw2, None, fn=lambda h, w2=w2, b=b, k=k: h.indirect_dma_start(
                    out=w2[:, k, :], out_offset=None, in_=self.w2,
                    in_offset=bass.IndirectOffsetOnAxis(ap=idxw[:, b, k:k + 1], axis=0)))
            sc.dma("pool", None, None, Bb1, None, fn=lambda h, b1_=b1_, b=b: h.indirect_dma_start(
                out=b1_, out_offset=None, in_=self.b1, in_offset=bass.IndirectOffsetOnAxis(ap=idxb1[:, b:b + 1], axis=0)))
            sc.dma("pool", None, None, Bb2, None, fn=lambda h, b2_=b2_, b=b: h.indirect_dma_start(
                out=b2_, out_offset=None, in_=self.b2, in_offset=bass.IndirectOffsetOnAxis(ap=idxb2[:, b:b + 1], axis=0)))
            b1p, Bb1p = b1ps[b % 2]
            sc.op("dve", lambda h, b1p=b1p, b1_=b1_: h.tensor_scalar(out=b1p, in0=b1_[:, 8:16], scalar1=1.0, scalar2=None, op0=ALU.add),
                  reads=[Bb1], writes=[Bb1p])
            if b == 0:
                prep_x(0)
            for j in range(8):
                pg, Bpg = self.psum()
                pl, Bpl = self.psum()
                for k in range(8):
                    self.mm(pg, Bpg, w1[:, k, j * 128:(j + 1) * 128], xT_[:, k, :], k == 0, k == 7, [Bw1, BxT])
                for k in range(8):
                    self.mm(pl, Bpl, w1[:, k, 1024 + j * 128:1024 + (j + 1) * 128], xT_[:, k, :], k == 0, k == 7, [Bw1, BxT])
                gc, Bgc = T_()
                sg, Bsg = T_()
                l1, Bl1 = T_()
                sc.op("dve", lambda h, gc=gc, pg=pg, j=j: h.tensor_scalar(out=gc, in0=pg, scalar1=b1_[:, j:j + 1], scalar2=SW_LIM,
                                                                         op0=ALU.add, op1=ALU.min), reads=Bpg + [Bb1], writes=[Bgc])
                sc.op("act", lambda h, gc=gc, sg=sg: h.activation(out=sg, in_=gc, func=AF.Sigmoid, scale=SW_ALPHA), reads=[Bgc], writes=[Bsg])
                sc.op("dve", lambda h, l1=l1, pl=pl, j=j: h.tensor_scalar(out=l1, in0=pl, scalar1=b1p[:, j:j + 1], scalar2=SW_LIM + 1.0,
                                                                         op0=ALU.add, op1=ALU.min), reads=Bpl + [Bb1p], writes=[Bl1])
                sc.op("dve", lambda h, gc=gc, sg=sg: h.tensor_tensor(out=gc, in0=gc, in1=sg, op=ALU.mult), reads=[Bgc, Bsg], writes=[Bgc])
                sc.op("dve", lambda h, gc=gc, l1=l1, j=j: h.scalar_tensor_tensor(out=aT[:, j, :], in0=l1, scalar=1.0 - SW_LIM, in1=gc,
                                                                                op0=ALU.max, op1=ALU.mult),
                      reads=[Bgc, Bl1], writes=[B_aT])
            if b + 1 < NBLK:
                prep_x(b + 1)
            for r in range(4):
                y_, By = yo[yc % 2]
                yc += 1
                for n in range(2):
                    py, Bpy = self.psum()
                    for j in range(8):
                        self.mm(py, Bpy, aT[:, j, r * 128:(r + 1) * 128], w2[:, j, n * 512:(n + 1) * 512], j == 0, j == 7, [B_aT, Bw2])
                    sc.op("dve", lambda h, y_=y_, py=py, n=n: h.tensor_tensor(out=y_[:, n * 512:(n + 1) * 512], in0=py,
                                                                              in1=b2_[:, n * 512:(n + 1) * 512], op=ALU.add),
                          reads=Bpy + [Bb2], writes=[By])
                r0 = b * BLK + r * 128
                sc.dma("sp", self.ys_d[r0:r0 + 128, :], y_, self.B_ys, By)
        sc.barrier()
        ar.release(m1)
        self.load_ln(self.lnp[l, 2], self.lnp[l, 3])
        y4 = [ar.alloc([128, 4, D], F32, "y4") for _ in range(2)]
        ht = [ar.alloc([128, D], F32, "cht") for _ in range(2)]
        zt = [ar.alloc([128, D], F32, "czt") for _ in range(2)]
        ot = [ar.alloc([128, D], F32, "cot") for _ in range(2)]
        stt = [ar.alloc([128, 16], F32, "cst") for _ in range(2)]
        dst_d, B_dst = (self.out, self.B_out) if last else (self.h_d, self.B_h)
        def gathers(tt):
            y_, By = y4[tt % 2]
            h_, Bh = ht[tt % 2]
            for k in range(4):
                sc.dma("pool", None, None, By, self.B_ys, fn=lambda h, y_=y_, tt=tt, k=k: h.indirect_dma_start(
                    out=y_[:, k, :], out_offset=None, in_=self.ys_d,
                    in_offset=bass.IndirectOffsetOnAxis(ap=idx4[:, tt, k:k + 1], axis=0)))
            sc.dma("sp", h_, self.h1_d[tt * 128:(tt + 1) * 128, :], Bh, self.B_h1)
        gathers(0)
        for tt in range(NTT):
            (y_, By), (h_, Bh), (z, Bz), (o, Bo), (st, Bst) = y4[tt % 2], ht[tt % 2], zt[tt % 2], ot[tt % 2], stt[tt % 2]
            if tt + 1 < NTT:
                gathers(tt + 1)
            sc.op("dve", lambda h, z=z, y_=y_, tt=tt: h.tensor_scalar(out=z, in0=y_[:, 0, :], scalar1=gate4[:, tt, 0:1], scalar2=None,
                                                                      op0=ALU.mult), reads=[By, B_g4], writes=[Bz])
            for k in range(1, 4):
                sc.op("dve", lambda h, z=z, y_=y_, tt=tt, k=k: h.scalar_tensor_tensor(out=z, in0=y_[:, k, :], scalar=gate4[:, tt, k:k + 1],
                                                                                     in1=z, op0=ALU.mult, op1=ALU.add),
                      reads=[By, B_g4, Bz], writes=[Bz])
            sc.op("dve", lambda h, z=z, h_=h_: h.scalar_tensor_tensor(out=z, in0=h_, scalar=DN_ALPHA, in1=z, op0=ALU.mult, op1=ALU.add),
                  reads=[Bh, Bz], writes=[Bz])
            self.ln_tile(z, Bz, o, Bo, st, Bst)
            sc.dma("sp", dst_d[tt * 128:(tt + 1) * 128, :], o, B_dst, Bo)
        sc.barrier()
        ar.release(m0)


def _kp(a, nk):
    L, R, N = a.shape
    return np.ascontiguousarray(a.reshape(L, nk, 128, N).transpose(0, 2, 1, 3))


def _pp(a, nt):
    L = a.shape[0]
    return np.ascontiguousarray(a.reshape(L, nt, 128).transpose(0, 2, 1))


def pack_shared(inp, depth, nexp=E):
    L = depth
    f = lambda k: np.asarray(inp[k], dtype=np.float32)[:L]
    d = {}
    d["ln_in"] = np.stack([np.asarray(inp["ln_in_g"], np.float32), np.asarray(inp["ln_in_b"], np.float32)])
    d["w_in"] = _kp(f("w_in"), 8)
    d["w_gate"] = _kp(f("w_gate"), 8)
    d["b_gate"] = _pp(f("b_gate"), 24)
    a = np.arange(128)[:, None, None]
    j = np.arange(5)[None, :, None]
    b = np.arange(128)[None, None, :]
    rel = (j - 4) * 128 + b - a
    qc = a // 64
    kc = (j - 4) * 2 + b // 64
    valid = (kc >= qc - 8) & (kc <= qc)
    idx = np.clip(rel, -128, 128) + 128
    rb = f("rel_bias")
    bq = rb[:, :, idx]
    bq = np.where(valid[None, None], bq, np.float32(NEG)).astype(np.float32)
    d["biasq"] = np.ascontiguousarray(bq.transpose(0, 2, 1, 3, 4)).reshape(L, 128, 8 * 5 * 128)
    cw = f("conv_w").transpose(0, 2, 1)
    d["conv_w"] = np.ascontiguousarray(cw.reshape(L, 4, 128, 31).transpose(0, 2, 1, 3)).reshape(L, 128, 124)
    d["conv_v"] = np.ascontiguousarray(np.concatenate([_pp(f("conv_b"), 4), _pp(f("conv_ln_g"), 4), _pp(f("conv_ln_b"), 4)], axis=2))
    d["w_pw2"] = _kp(f("w_pw2"), 4)
    d["qkv_g"] = np.ascontiguousarray(np.concatenate([_pp(f("q_norm_g"), 3), _pp(f("kv_norm_g"), 2)], axis=2))
    d["w_uq"] = _kp(f("w_uq"), 3)
    d["w_ukv"] = _kp(f("w_ukv"), 2)
    d["w_oa"] = _kp(f("w_oa"), 4)
    d["w_oc"] = _kp(f("w_oc"), 4)
    d["w_out"] = _kp(f("w_out"), 8)
    d["lnp"] = np.ascontiguousarray(np.stack([f("ln1_g"), f("ln1_b"), f("ln2_g"), f("ln2_b")], axis=1))
    d["w_router"] = _kp(f("w_router")[:, :, :nexp], 8).reshape(L, 128, 8 * nexp)
    d["b_router"] = np.ascontiguousarray(f("b_router")[:, :nexp])
    d["w1"] = np.ascontiguousarray(f("w1")[:, :nexp]).reshape(L * nexp * 1024, 2048)
    b1 = f("b1")[:, :nexp]
    d["b1"] = np.ascontiguousarray(b1.reshape(L, nexp, 16, 128).transpose(0, 1, 3, 2)).reshape(L * nexp * 128, 16)
    d["w2"] = np.ascontiguousarray(f("w2")[:, :nexp]).reshape(L * nexp * 1024, 1024)
    d["b2"] = np.ascontiguousarray(f("b2")[:, :nexp]).reshape(L * nexp, 1024)
    cv = np.zeros((128, 8), np.float32)
    inv = (10000.0 ** (-np.arange(0, 32, 2, dtype=np.float32) / 32)).astype(np.float32)
    cv[64:96, 0] = np.concatenate([inv, inv])
    cv[:, 1] = -math.pi
    cv[:, 6] = LN_EPS
    cv[:, 7] = RMS_EPS
    d["cvec"] = cv
    return d


def pack_core(inp, seqs):
    x = np.asarray(inp["x"], np.float32)[seqs].reshape(-1, D)
    pos = np.asarray(inp["positions"], np.int32)[seqs].reshape(-1)
    return {"x": np.ascontiguousarray(x), "posrep": np.ascontiguousarray(np.broadcast_to(pos[None, :], (32, pos.shape[0])))}


_CACHE = {}


def kernel(**inputs):
    n = 8
    nseq = 4
    if "nc" not in _CACHE:
        _CACHE["nc"] = Prog(nseq, 4).build()
    nc = _CACHE["nc"]
    shared = pack_shared(inputs, 4)
    in_maps = []
    for c in range(n):
        d = dict(shared)
        d.update(pack_core(inputs, list(range(c * nseq, (c + 1) * nseq))))
        in_maps.append(d)
    res = run_bass_kernel_spmd(nc, in_maps, core_ids=list(range(n)))
    out = np.concatenate([r["out"] for r in res.results], axis=0)
    return out.reshape(32, S, D).astype(np.float32)
```
